# Optimizing a Trainium2 kernel written in Bass

```python
import jax, jax.numpy as jnp
from jax import lax
import numpy as np

D_MODEL = 1024
BATCH = 8
SEQ = 8192
DEPTH = 1

EPS = 1e-6
D_FF = 2816
A_WIDTH = 1024
A_GROUPS = 8
A_GROUP_DIM = A_WIDTH // A_GROUPS
A_CHUNK = 128
B_HEADS = 4
B_KEY_DIM = D_MODEL // 2
B_VAL_DIM = D_MODEL
B_HEAD_K = B_KEY_DIM // B_HEADS
B_HEAD_V = B_VAL_DIM // B_HEADS
B_GATE_RANK = 16
B_GATE_TAU = 16.0
B_CHUNK = 64
IN_SIZES = (A_WIDTH, A_WIDTH,
            B_KEY_DIM, B_KEY_DIM,
            B_VAL_DIM, B_VAL_DIM,
            B_GATE_RANK,
            2 * D_MODEL)
D_IN = sum(IN_SIZES)

kernel_name = "hybrid_gmlp_gla_macaron_block"


def _rmsnorm(x, g):
    xf = x.astype(jnp.float32)
    y = xf * lax.rsqrt(jnp.mean(xf * xf, axis=-1, keepdims=True) + EPS)
    return (y * g.astype(jnp.float32)).astype(x.dtype)


def _layernorm(x, g, b):
    xf = x.astype(jnp.float32)
    mu = jnp.mean(xf, axis=-1, keepdims=True)
    var = jnp.mean(jnp.square(xf - mu), axis=-1, keepdims=True)
    y = (xf - mu) * lax.rsqrt(var + EPS)
    return (y * g.astype(jnp.float32) + b.astype(jnp.float32)).astype(x.dtype)


def _swiglu(x, w_in, w_out):
    a, b = jnp.split(x @ w_in, 2, axis=-1)
    return (jax.nn.silu(a) * b) @ w_out


def _split(z, sizes):
    idx = [int(i) for i in np.cumsum(sizes)[:-1]]
    return jnp.split(z, idx, axis=-1)


def _gmlp_mixer(u, v, ln_g, ln_b, w_s, b_s):
    bsz, s, _ = u.shape
    u = jax.nn.gelu(u, approximate=False)
    v = _layernorm(jax.nn.gelu(v, approximate=False), ln_g, ln_b)
    v = v.reshape(bsz, s // A_CHUNK, A_CHUNK, A_GROUPS, A_GROUP_DIM)
    mask = jnp.tril(jnp.ones((A_CHUNK, A_CHUNK), dtype=bool))
    ws = jnp.where(mask[None], w_s, jnp.zeros_like(w_s))
    sp = jnp.einsum('gij,bnjgc->bnigc', ws, v) + b_s.T[None, None, :, :, None]
    return u * sp.reshape(bsz, s, A_WIDTH)


def _gla_mixer(q, k, v, r, a_lr, w_alpha, b_alpha, head_g):
    bsz, s, _ = q.shape
    n = s // B_CHUNK
    f32 = jnp.float32
    log_a = jax.nn.log_sigmoid((a_lr @ w_alpha + b_alpha).astype(f32)) / B_GATE_TAU

    def heads(t, d):
        return t.astype(f32).reshape(bsz, n, B_CHUNK, B_HEADS, d).transpose(0, 3, 1, 2, 4)

    qh = heads(q, B_HEAD_K) * (B_HEAD_K ** -0.5)
    kh = heads(k, B_HEAD_K)
    vh = heads(v, B_HEAD_V)
    bcum = jnp.cumsum(heads(log_a, B_HEAD_K), axis=3)
    b_mid = bcum[:, :, :, B_CHUNK // 2 - 1:B_CHUNK // 2, :]
    b_last = bcum[:, :, :, -1, :]

    q_in = qh * jnp.exp(bcum - b_mid)
    k_in = kh * jnp.exp(b_mid - bcum)
    scores = jnp.einsum('bhnik,bhnjk->bhnij', q_in, k_in)
    cmask = jnp.tril(jnp.ones((B_CHUNK, B_CHUNK), dtype=bool))
    scores = jnp.where(cmask, scores, 0.0)
    o_intra = jnp.einsum('bhnij,bhnjv->bhniv', scores, vh)

    q_out = qh * jnp.exp(bcum)
    k_st = kh * jnp.exp(b_last[:, :, :, None, :] - bcum)
    decay = jnp.exp(b_last)

    def step(state, xs):
        qn, kn, vn, dn = xs
        o = jnp.einsum('bhik,bhkv->bhiv', qn, state)
        state = dn[..., None] * state + jnp.einsum('bhjk,bhjv->bhkv', kn, vn)
        return state, o

    s0 = jnp.zeros((bsz, B_HEADS, B_HEAD_K, B_HEAD_V), f32)
    xs = (jnp.moveaxis(q_out, 2, 0), jnp.moveaxis(k_st, 2, 0),
          jnp.moveaxis(vh, 2, 0), jnp.moveaxis(decay, 2, 0))
    _, o_inter = lax.scan(step, s0, xs)
    o = o_intra + jnp.moveaxis(o_inter, 0, 2)
    o = o.transpose(0, 2, 3, 1, 4).reshape(bsz, s, B_HEADS, B_HEAD_V)
    o = o * lax.rsqrt(jnp.mean(o * o, axis=-1, keepdims=True) + EPS) * head_g.astype(f32)
    o = o.reshape(bsz, s, B_VAL_DIM).astype(v.dtype)
    return o * jax.nn.silu(r)


def setup_inputs(seed: int = 0) -> dict:
    key = jax.random.key(seed)
    ks = iter(jax.random.split(key, 32))
    L = DEPTH

    def w(shape, fan_in):
        return jax.random.normal(next(ks), shape, jnp.float32) * fan_in ** -0.5

    def gain(shape):
        return 1.0 + 0.1 * jax.random.normal(next(ks), shape, jnp.float32)

    def small(shape, scale):
        return scale * jax.random.normal(next(ks), shape, jnp.float32)

    return {
        "x": jax.random.normal(next(ks), (BATCH, SEQ, D_MODEL), jnp.float32),
        "ffn1_norm": gain((L, D_MODEL)),
        "ffn1_w_in": w((L, D_MODEL, 2 * D_FF), D_MODEL),
        "ffn1_w_out": w((L, D_FF, D_MODEL), D_FF),
        "mix_norm": gain((L, D_MODEL)),
        "w_in": w((L, D_MODEL, D_IN), D_MODEL),
        "b_gate": small((L, 2 * D_MODEL), 0.1),
        "a_ln_gain": gain((L, A_WIDTH)),
        "a_ln_bias": small((L, A_WIDTH), 0.02),
        "a_w_s": w((L, A_GROUPS, A_CHUNK, A_CHUNK), A_CHUNK),
        "a_b_s": gain((L, A_GROUPS, A_CHUNK)),
        "b_w_alpha": w((L, B_GATE_RANK, B_KEY_DIM), B_GATE_RANK),
        "b_b_alpha": small((L, B_KEY_DIM), 0.5),
        "b_head_norm": gain((L, B_HEADS, B_HEAD_V)),
        "w_proj_a": w((L, A_WIDTH, D_MODEL), A_WIDTH),
        "w_proj_b": w((L, B_VAL_DIM, D_MODEL), B_VAL_DIM),
        "w_o": w((L, D_MODEL, D_MODEL), D_MODEL),
        "ffn2_norm": gain((L, D_MODEL)),
        "ffn2_w_in": w((L, D_MODEL, 2 * D_FF), D_MODEL),
        "ffn2_w_out": w((L, D_FF, D_MODEL), D_FF),
        "final_norm": gain((D_MODEL,)),
    }


def reference(x, ffn1_norm, ffn1_w_in, ffn1_w_out, mix_norm, w_in, b_gate,
              a_ln_gain, a_ln_bias, a_w_s, a_b_s, b_w_alpha, b_b_alpha, b_head_norm,
              w_proj_a, w_proj_b, w_o, ffn2_norm, ffn2_w_in, ffn2_w_out, final_norm):
    for l in range(DEPTH):
        x = x + 0.5 * _swiglu(_rmsnorm(x, ffn1_norm[l]), ffn1_w_in[l], ffn1_w_out[l])

        h = _rmsnorm(x, mix_norm[l])
        z = h @ w_in[l]
        u_a, v_a, q_b, k_b, v_b, r_b, alr_b, g = _split(z, IN_SIZES)
        gate_a, gate_b = jnp.split(jax.nn.sigmoid(g + b_gate[l]), 2, axis=-1)

        y_a = _gmlp_mixer(u_a, v_a, a_ln_gain[l], a_ln_bias[l], a_w_s[l], a_b_s[l]) @ w_proj_a[l]
        y_b = _gla_mixer(q_b, k_b, v_b, r_b, alr_b, b_w_alpha[l], b_b_alpha[l],
                         b_head_norm[l]) @ w_proj_b[l]
        x = x + (gate_a * y_a + gate_b * y_b) @ w_o[l]

        x = x + 0.5 * _swiglu(_rmsnorm(x, ffn2_norm[l]), ffn2_w_in[l], ffn2_w_out[l])
    return _rmsnorm(x, final_norm)
```

```python
import numpy as np
from contextlib import ExitStack
import concourse.bass as bass
import concourse.mybir as mybir
from concourse.bass_utils import run_bass_kernel_spmd

F32 = mybir.dt.float32
BF16 = mybir.dt.bfloat16
AF = mybir.ActivationFunctionType
ALU = mybir.AluOpType

D = 1024
DFF = 2816
NF = DFF // 128
DIN = 7184
T = 512
NSUB = T // 128
SEQ = 8192
EPS = 1e-6
EPOCH = 30000
QSCALE = 128 ** -0.5

C_U, C_VA, C_Q, C_K, C_VB, C_R, C_ALR, C_G = 0, 1024, 2048, 2560, 3072, 4096, 5120, 5136


class Op:
    __slots__ = ("eng", "fn", "deps", "idx", "signal", "sigidx", "dma", "sem", "semval")


class Prog:
    ENGS = ("pe", "act", "dve", "pool", "sp")

    def __init__(self):
        self.ops = []
        self.lw = {}
        self.rd = {}
        self.dma_cnt = {}

    def add(self, eng, fn, reads=(), writes=(), dma_sem=None):
        i = len(self.ops)
        deps = set()
        for k in reads:
            w = self.lw.get(k)
            if w is not None:
                deps.add(w)
        for k in writes:
            w = self.lw.get(k)
            if w is not None:
                deps.add(w)
            r = self.rd.get(k)
            if r:
                deps.update(r)
        for k in writes:
            self.lw[k] = i
            self.rd[k] = []
        for k in reads:
            if k in self.rd:
                self.rd[k].append(i)
            else:
                self.rd[k] = [i]
        deps.discard(i)
        op = Op()
        op.eng, op.fn, op.deps, op.idx = eng, fn, deps, i
        op.signal, op.sigidx = False, -1
        op.dma = dma_sem is not None
        op.sem, op.semval = None, 0
        if op.dma:
            c = self.dma_cnt.get(dma_sem, 0) + 1
            self.dma_cnt[dma_sem] = c
            op.sem, op.semval = dma_sem, 16 * c
        self.ops.append(op)
        return i

    def finalize(self):
        ops = self.ops
        for op in ops:
            keep = set()
            for d in op.deps:
                o = ops[d]
                if o.eng == op.eng and (op.eng == "pe" or (o.dma and op.dma)):
                    continue
                keep.add(d)
            op.deps = keep
            for d in keep:
                if not ops[d].dma:
                    ops[d].signal = True
        cnt = {e: 0 for e in self.ENGS}
        for op in ops:
            if op.signal:
                op.sigidx = cnt[op.eng]
                cnt[op.eng] += 1
        self.sig_cnt = cnt

    def emit(self, nc, es, final_wait_sems=()):
        self.finalize()
        ops = self.ops
        eng_sems = {}
        for e in self.ENGS:
            n = self.sig_cnt[e] // EPOCH + 1
            eng_sems[e] = [es.enter_context(nc.semaphore(f"e_{e}_{k}")) for k in range(n)]
        dma_sems = {name: es.enter_context(nc.semaphore(f"d_{name}")) for name in self.dma_cnt}
        per_eng = {e: [op for op in ops if op.eng == e] for e in self.ENGS}
        block = es.enter_context(nc.Block())

        def run(engname, e):
            waited = {}
            for op in per_eng[engname]:
                need = {}
                for d in op.deps:
                    o = ops[d]
                    if o.dma:
                        key = ("d", o.sem)
                        val = o.semval
                        sem = dma_sems[o.sem]
                    else:
                        ep = o.sigidx // EPOCH
                        key = ("e", o.eng, ep)
                        val = o.sigidx % EPOCH + 1
                        sem = eng_sems[o.eng][ep]
                    if val > need.get(key, (0, None))[0]:
                        need[key] = (val, sem)
                for key, (val, sem) in need.items():
                    if waited.get(key, 0) >= val:
                        continue
                    waited[key] = val
                    e.wait_ge(sem, val)
                ins = op.fn(e)
                if op.dma:
                    ins.then_inc(dma_sems[op.sem], 16)
                elif op.signal:
                    ins.then_inc(eng_sems[engname][op.sigidx // EPOCH], 1)
            if engname == "pool":
                for name in final_wait_sems:
                    if name in self.dma_cnt:
                        e.wait_ge(dma_sems[name], 16 * self.dma_cnt[name])

        @block.tensor
        def _(e):
            run("pe", e)

        @block.scalar
        def _(e):
            run("act", e)

        @block.vector
        def _(e):
            run("dve", e)

        @block.gpsimd
        def _(e):
            run("pool", e)

        @block.sync
        def _(e):
            run("sp", e)


def build_nc(n_tok=SEQ):
    assert n_tok % T == 0
    n_pass = n_tok // T
    nc = bass.Bass("TRN2", target_bir_lowering=False)
    P = Prog()
    es = ExitStack()

    def dram_in(name, shape):
        return nc.dram_tensor(name, list(shape), F32, kind="ExternalInput").ap()

    x_d = dram_in("x", (n_tok, D))
    f1i_d = dram_in("ffn1_w_in", (D, 2 * DFF))
    f1o_d = dram_in("ffn1_w_out", (DFF, D))
    win_d = dram_in("w_in", (D, DIN))
    wpa_d = dram_in("w_proj_a", (D, D))
    wpb_d = dram_in("w_proj_b", (D, D))
    wo_d = dram_in("w_o", (D, D))
    f2i_d = dram_in("ffn2_w_in", (D, 2 * DFF))
    f2o_d = dram_in("ffn2_w_out", (DFF, D))
    cols_d = dram_in("cols", (128, 64))
    rows_d = dram_in("rows_bc", (128, 2048))
    wst_d = dram_in("wst", (128, 1024))
    bs2_d = dram_in("bs2", (2, 1024))
    wal_d = dram_in("wal18", (18, 512))
    ident_d = dram_in("ident", (128, 128))
    umask_d = dram_in("umask", (128, 128))
    y_d = nc.dram_tensor("y", [n_tok, D], F32, kind="ExternalOutput").ap()

    def scr(name, g, n):
        return nc.dram_tensor(name, [g, 128, n], BF16, kind="Internal").ap()

    s_f1i = scr("s_f1i", 11, 4096)
    s_f1o = scr("s_f1o", 8, NF * 128)
    s_mw = scr("s_mw", 20, 4096)
    s_f2i = scr("s_f2i", 11, 4096)
    s_f2o = scr("s_f2o", 8, NF * 128)
    MW = {n: i for i, n in enumerate(
        ["u0", "u1", "va0", "va1", "q", "k", "vb0", "vb1", "r0", "r1",
         "g0", "g1", "g2", "g3", "p0", "p1", "p2", "p3", "wo0", "wo1"])}

    def sb(name, shape, dt):
        return es.enter_context(nc.sbuf_tensor("sb_" + name, list(shape), dt))

    xT = sb("xT", (128, 8, T), F32)
    hT = sb("hT", (128, 8, T), BF16)
    Bt = sb("Bt", (128, NF, T), BF16)
    at = sb("at", (128, 3, T), F32)
    sq = sb("sq", (128, 2, T), BF16)
    xin = sb("xin", (128, 2, D), F32)
    ost = sb("ost", (128, 2, T), F32)
    ring = sb("ring", (128, 4, 4096), BF16)
    gbc = sb("gbc", (128, 2, D), F32)
    vbm = sb("vbm", (128, 4, D), BF16)
    EE = sb("EE", (128, 4, T), F32)
    kptm = sb("kptm", (128, 2, 512), BF16)
    scm = sb("scm", (128, 2, 512), BF16)
    S = sb("S", (128, 4, 256), F32)
    Sbf = sb("Sbf", (128, 4, 256), BF16)
    ogT = sb("ogT", (128, 8, T), BF16)
    G = sb("G", (128, 2, D), F32)
    vn = sb("vn", (128, 2, D), BF16)
    alr = sb("alr", (18, T), BF16)
    ident_f = sb("ident_f", (128, 128), F32)
    ident_b = sb("ident_b", (128, 128), BF16)
    U_f = sb("U_f", (128, 128), F32)
    U16 = sb("U16", (128, 128), BF16)
    mask4 = sb("mask4", (128, 4, 128), BF16)
    WsTm = sb("WsTm", (128, 8, 128), BF16)
    bs2hl = sb("bs2hl", (2, D), BF16)
    wal18b = sb("wal18b", (18, 512), BF16)
    walr = sb("walr", (128, 8, 16), BF16)
    ones_b = sb("ones_b", (128, 128), BF16)
    cols = sb("cols", (128, 64), F32)
    small = sb("small", (128, 128), F32)

    ps = [es.enter_context(nc.psum_tensor(f"ps{b}", [128, 512], F32)) for b in range(8)]

    CN_F1, CN_MX, CN_F2, CN_FIN, CN_BG, CN_HG = 0, 8, 16, 24, 32, 48
    eps_c = cols[:, 60:61]
    one_c = cols[:, 61:62]
    bgh = small[:, 0:16]
    hgh = small[:, 16:24]
    negh = small[:, 24:40]
    dcol = small[:, 40:56]
    bnst4 = small[:, 56:80]
    mv4 = small[:, 80:88]
    msq = small[:, 88:92]
    rsto = small[:, 92:96]
    bnst = small[:, 96:108]
    mv = small[:, 108:110]
    rln = small[:, 110:111]

    def og_v(b):
        return EE[:, b, :].bitcast(BF16)

    def t1_v(b):
        return EE[:, 2 + b, :]

    rstd_t = EE[:, 2, :]

    def uT(m):
        return Bt[:, m, :]

    def qo(h):
        return Bt[:, 8 + h, :]

    def kp(h):
        return Bt[:, 12 + h, :]

    def lv(s):
        return Bt[:, 16 + s, :]

    vbm_flat = vbm[:].rearrange("p s n -> p (s n)")

    def mT(m):
        return vbm_flat[:, m * T:(m + 1) * T]

    def psb(b):
        return ps[b][:].bitcast(BF16)

    state = {"at": 0, "wcnt": 0, "xin": 0, "ost": 0}

    def next_at():
        i = state["at"] % 3
        state["at"] += 1
        return i

    def mm_op(out_ap, pairs, reads, writes):
        pairs = list(pairs)

        def fn(e):
            n = len(pairs)
            ins = None
            for i, (l, r) in enumerate(pairs):
                ins = e.matmul(out_ap, l, r, start=(i == 0), stop=(i == n - 1))
            return ins
        P.add("pe", fn, reads, writes)

    def mm_multi(items, reads, writes):
        items = [(o, list(pp)) for o, pp in items]

        def fn(e):
            ins = None
            for o, pp in items:
                n = len(pp)
                for i, (l, r) in enumerate(pp):
                    ins = e.matmul(o, l, r, start=(i == 0), stop=(i == n - 1))
            return ins
        P.add("pe", fn, reads, writes)

    def tr_multi(items, reads, writes):
        items = list(items)

        def fn(e):
            ins = None
            for o, i_, idn in items:
                ins = e.transpose(o, i_, idn)
            return ins
        P.add("pe", fn, reads, writes)

    def stream(src_ap, n, scr_key):
        slot = state["wcnt"] % 4
        state["wcnt"] += 1
        dst = ring[:, slot, 0:n]
        P.add("sp", lambda e: e.dma_start(out=dst, in_=src_ap), reads=[("scr", scr_key)],
              writes=[("W", slot)], dma_sem=f"w{slot}")
        return slot

    def wview(slot, k, n):
        return ring[:, slot, 0:k * n].rearrange("p (k n) -> p k n", k=k)

    HT_ALL = [("hT", c) for c in range(8)]

    def setup():
        cl = "c"

        def ld(dst, src, key):
            P.add("sp", lambda e: e.dma_start(out=dst, in_=src), writes=[key], dma_sem=cl)
        ld(cols[:], cols_d, ("cols",))
        ld(ident_f[:], ident_d, ("ident_f",))
        ld(U_f[:], umask_d, ("U_f",))
        ld(gbc[:].rearrange("p a n -> p (a n)"), rows_d, ("gbc",))
        wst_st = at[:].rearrange("p a n -> p (a n)")[:, 0:1024]
        P.add("sp", lambda e: e.dma_start(out=wst_st, in_=wst_d),
              writes=[("at", 0), ("at", 1)], dma_sem=cl)
        bs2_st = G[0:2, 0, :]
        P.add("sp", lambda e: e.dma_start(out=bs2_st, in_=bs2_d), writes=[("G", 0, 0), ("G", 0, 1)], dma_sem=cl)
        wal_st = G[0:18, 1, 0:512]
        P.add("sp", lambda e: e.dma_start(out=wal_st, in_=wal_d), writes=[("G", 1, 0)], dma_sem=cl)
        fence_keys = [("cols",), ("ident_f",), ("U_f",), ("gbc",), ("at", 0), ("at", 1), ("G", 0, 0), ("G", 0, 1), ("G", 1, 0)]
        P.add("sp", lambda e: e.dma_start(out=small[0:1, 112:128], in_=cols_d[0:1, 0:16]),
              reads=[], writes=fence_keys + [("fence",)], dma_sem=cl)

        P.add("pool", lambda e: e.dma_start(
            out=walr[:], in_=win_d[:, C_ALR:C_ALR + 16].rearrange("(k p) n -> p k n", p=128)),
            writes=[("walr",)], dma_sem="walr")

        P.add("dve", lambda e: e.tensor_copy(out=ident_b[:], in_=ident_f[:]),
              reads=[("ident_f",)], writes=[("ident_b",)])
        P.add("dve", lambda e: e.tensor_scalar(out=U16[:], in0=U_f[:], scalar1=-1.0 / 16.0, scalar2=None,
                                               op0=ALU.mult), reads=[("U_f",)], writes=[("U16",)])

        def m4(e):
            ins = None
            for h in range(4):
                ins = e.tensor_copy(out=mask4[:, h, :], in_=U_f[:])
            return ins
        P.add("dve", m4, reads=[("U_f",)], writes=[("mask4",)])

        def wsm(e):
            ins = None
            for g in range(8):
                ins = e.tensor_tensor(out=WsTm[:, g, :], in0=wst_st[:, g * 128:(g + 1) * 128], in1=U_f[:],
                                      op=ALU.mult)
            return ins
        P.add("dve", wsm, reads=[("at", 0), ("at", 1), ("U_f",)], writes=[("WsTm",)])

        P.add("dve", lambda e: e.memset(ones_b[:], 1.0), writes=[("ones",)])
        P.add("dve", lambda e: e.memset(alr[:], 1.0), writes=[("alr",)])
        P.add("dve", lambda e: e.memset(S[:].rearrange("p h n -> p (h n)"), 0.0), writes=[("S",)])
        P.add("dve", lambda e: e.memset(Sbf[:].rearrange("p h n -> p (h n)"), 0.0), writes=[("Sbf",)])
        P.add("dve", lambda e: e.memset(negh, -0.5), writes=[("negh",)])
        P.add("dve", lambda e: e.tensor_scalar(out=bgh, in0=cols[:, CN_BG:CN_BG + 16], scalar1=0.5,
                                               scalar2=None, op0=ALU.mult),
              reads=[("cols",)], writes=[("bgh",)])
        P.add("dve", lambda e: e.tensor_scalar(out=hgh, in0=cols[:, CN_HG:CN_HG + 8], scalar1=0.5,
                                               scalar2=None, op0=ALU.mult),
              reads=[("cols",)], writes=[("hgh",)])

        def hilo(dst, st, np_, n, selA, selD, tmpA, tmpD, rk, wk):
            allk = rk + wk + [("cols",)]
            P.add("dve", lambda e: e.tensor_copy(out=dst, in_=st), reads=allk, writes=wk)
            P.add("dve", lambda e: e.tensor_tensor(out=tmpD, in0=st, in1=dst, op=ALU.subtract),
                  reads=allk, writes=wk)
            P.add("dve", lambda e: e.tensor_scalar(out=tmpD, in0=tmpD, scalar1=selD, scalar2=None, op0=ALU.mult),
                  reads=allk, writes=wk)
            P.add("dve", lambda e: e.tensor_copy(out=tmpA, in_=dst), reads=allk, writes=wk)
            P.add("dve", lambda e: e.scalar_tensor_tensor(out=dst, in0=tmpA, scalar=selA, in1=tmpD,
                                                          op0=ALU.mult, op1=ALU.add), reads=allk, writes=wk)
        hilo(bs2hl[:], bs2_st, 2, 1024, cols[0:2, 58:59], cols[0:2, 59:60],
             EE[0:2, 0:2, :].rearrange("p a n -> p (a n)"), EE[0:2, 2:4, :].rearrange("p a n -> p (a n)"),
             [("G", 0, 0), ("G", 0, 1)], [("bs2hl",), ("EE", 0), ("EE", 1), ("EE", 2), ("EE", 3)])
        hilo(wal18b[:], wal_st, 18, 512, cols[0:18, 56:57], cols[0:18, 57:58],
             EE[0:18, 0, :], EE[0:18, 2, :],
             [("G", 1, 0)], [("wal18b",), ("EE", 0), ("EE", 2)])

        def conv(dst, src, key, sem):
            P.add("pool", lambda e: e.dma_start(out=dst, in_=src), writes=[("scr", key)], dma_sem=sem)

        def conv_ffn(sin, sout, wi, wo_, key_i, key_o):
            for j in range(11):
                dv = sin[j].rearrange("p (k n) -> p k n", k=8)
                a0 = 2 * j * 128
                conv(dv[:, :, 0:256], wi[:, a0:a0 + 256].rearrange("(k p) n -> p k n", p=128),
                     (key_i, j), f"{key_i}_{j}")
                conv(dv[:, :, 256:512], wi[:, DFF + a0:DFF + a0 + 256].rearrange("(k p) n -> p k n", p=128),
                     (key_i, j), f"{key_i}_{j}")
            for m in range(8):
                dv = sout[m].rearrange("p (k n) -> p k n", k=NF)
                conv(dv, wo_[:, m * 128:(m + 1) * 128].rearrange("(k p) n -> p k n", p=128),
                     (key_o, m), f"{key_o}_{m}")

        conv_ffn(s_f1i, s_f1o, f1i_d, f1o_d, "f1i", "f1o")

        def conv_cols(name, src, c0, n=512, off=0):
            dv = s_mw[MW[name]].rearrange("p (k n) -> p k n", k=8)
            conv(dv[:, :, off:off + n], src[:, c0:c0 + n].rearrange("(k p) n -> p k n", p=128),
                 ("mw", name), f"mw_{name}")
        conv_cols("vb0", win_d, C_VB)
        conv_cols("vb1", win_d, C_VB + 512)
        conv_cols("q", win_d, C_Q)
        conv_cols("k", win_d, C_K)
        conv_cols("r0", win_d, C_R)
        conv_cols("r1", win_d, C_R + 512)
        conv_cols("u0", win_d, C_U)
        conv_cols("u1", win_d, C_U + 512)
        conv_cols("va0", win_d, C_VA)
        conv_cols("va1", win_d, C_VA + 512)
        for j in range(4):
            conv_cols(f"g{j}", win_d, C_G + 256 * j, 256, 0)
            conv_cols(f"g{j}", win_d, C_G + 1024 + 256 * j, 256, 256)
            conv_cols(f"p{j}", wpa_d, 256 * j, 256, 0)
            conv_cols(f"p{j}", wpb_d, 256 * j, 256, 256)
        conv_cols("wo0", wo_d, 0)
        conv_cols("wo1", wo_d, 512)
        conv_ffn(s_f2i, s_f2o, f2i_d, f2o_d, "f2i", "f2o")

    def issue_xload(p, s):
        b = state["xin"] % 2
        state["xin"] += 1
        t0 = p * T + s * 128
        dst = xin[:, b, :]
        src = x_d[t0:t0 + 128, :]
        P.add("sp", lambda e: e.dma_start(out=dst, in_=src), writes=[("xin", b)], dma_sem=f"xin{b}")
        return b

    def x_transpose(s, b):
        for half in range(2):
            bank = half
            tr_multi([(ps[bank][:, j * 128:(j + 1) * 128], xin[:, b, (4 * half + j) * 128:(4 * half + j + 1) * 128],
                       ident_f[:]) for j in range(4)],
                     reads=[("xin", b), ("ident_f",)], writes=[("ps", bank)])
            dst = xT[:, 4 * half:4 * half + 4, s * 128:(s + 1) * 128]
            src = ps[bank][:].rearrange("p (j n) -> p j n", j=4)
            wk = [("xT", c) for c in range(4 * half, 4 * half + 4)]
            if half == 0:
                P.add("act", lambda e, dst=dst, src=src: e.copy(out=dst, in_=src), reads=[("ps", bank)], writes=wk)
            else:
                P.add("dve", lambda e, dst=dst, src=src: e.tensor_copy(out=dst, in_=src), reads=[("ps", bank)],
                      writes=wk)

    def norm_sq(c):
        i = c % 2
        P.add("act", lambda e: e.activation(out=sq[:, i, :], in_=xT[:, c, :], func=AF.Square),
              reads=[("xT", c)], writes=[("sq", i)])

    def norm_mm(c):
        i = c % 2

        def fn(e):
            return e.matmul(ps[6][:], ones_b[:], sq[:, i, :], start=(c == 0), stop=(c == 7))
        P.add("pe", fn, reads=[("sq", i), ("ones",)], writes=[("ps", 6)])

    def norm_finish(cn, final=False):
        P.add("act", lambda e: e.activation(out=small[:, 113:114], in_=cols[:, 61:62], func=AF.Sqrt),
              reads=[("cols",)], writes=[("dummy",)])
        P.add("act", lambda e: e.activation(out=rstd_t, in_=ps[6][:], func=AF.Sqrt, bias=eps_c,
                                            scale=1.0 / D),
              reads=[("ps", 6), ("cols",)], writes=[("EE", 2)])
        P.add("dve", lambda e: e.reciprocal(out=rstd_t, in_=rstd_t), reads=[("EE", 2)], writes=[("EE", 2)])
        for c in range(8):
            if final:
                P.add("dve", lambda e, c=c: e.scalar_tensor_tensor(
                    out=xT[:, c, :], in0=xT[:, c, :], scalar=cols[:, cn + c:cn + c + 1], in1=rstd_t,
                    op0=ALU.mult, op1=ALU.mult),
                    reads=[("xT", c), ("EE", 2), ("cols",)], writes=[("xT", c)])
            else:
                P.add("dve", lambda e, c=c: e.scalar_tensor_tensor(
                    out=hT[:, c, :], in0=xT[:, c, :], scalar=cols[:, cn + c:cn + c + 1], in1=rstd_t,
                    op0=ALU.mult, op1=ALU.mult),
                    reads=[("xT", c), ("EE", 2), ("cols",)], writes=[("hT", c)])

    def ffn(s_in, s_out, key_i, key_o):
        cnt = 0
        for j in range(11):
            slot = stream(s_in[j], 4096, (key_i, j))
            W = wview(slot, 8, 512)
            ai = {}
            order = (("a", 0), ("b", 0), ("a", 1), ("b", 1))
            if j == 0:
                for kc in range(8):
                    for q_, (kind, jj) in enumerate(order):
                        mi = jj if kind == "a" else 2 + jj
                        P.add("pe", lambda e, kc=kc, q_=q_, mi=mi, W=W: e.matmul(
                            ps[q_][:], W[:, kc, mi * 128:(mi + 1) * 128], hT[:, kc, :],
                            start=(kc == 0), stop=(kc == 7)),
                            reads=[("W", slot), ("hT", kc)], writes=[("ps", q_)])
            for (kind, jj) in order:
                mi = jj if kind == "a" else 2 + jj
                bank = cnt % 4
                cnt += 1
                if j > 0:
                    mm_op(ps[bank][:], [(W[:, kc, mi * 128:(mi + 1) * 128], hT[:, kc, :]) for kc in range(8)],
                          reads=[("W", slot)] + HT_ALL, writes=[("ps", bank)])
                if kind == "a":
                    i = next_at()
                    ai[jj] = i
                    P.add("act", lambda e, i=i, bank=bank: e.activation(out=at[:, i, :], in_=ps[bank][:],
                                                                         func=AF.Silu),
                          reads=[("ps", bank)], writes=[("at", i)])
                else:
                    i = ai[jj]
                    c = 2 * j + jj
                    P.add("dve", lambda e, i=i, bank=bank, c=c: e.tensor_tensor(
                        out=Bt[:, c, :], in0=ps[bank][:], in1=at[:, i, :], op=ALU.mult),
                        reads=[("ps", bank), ("at", i)], writes=[("B", c)])
        for m in range(8):
            slot = stream(s_out[m], NF * 128, (key_o, m))
            Wo = wview(slot, NF, 128)
            bank = 4 + m % 2
            mm_op(ps[bank][:], [(Wo[:, kc, :], Bt[:, kc, :]) for kc in range(NF)],
                  reads=[("W", slot)] + [("B", c) for c in range(NF)], writes=[("ps", bank)])
            if m > 0:
                norm_mm(m - 1)
            P.add("dve", lambda e, m=m, bank=bank: e.scalar_tensor_tensor(
                out=xT[:, m, :], in0=ps[bank][:], scalar=0.5, in1=xT[:, m, :], op0=ALU.mult, op1=ALU.add),
                reads=[("ps", bank), ("xT", m)], writes=[("xT", m)])
            norm_sq(m)
        norm_mm(7)

    def mixer(p):
        use_pool = p > 0

        def rsqrt_small(ap, n, key):
            if use_pool:
                P.add("pool", lambda e: e.tensor_tensor(out=ap, in0=ap, in1=negh[:, 0:n], op=ALU.pow),
                      reads=[key, ("negh",)], writes=[key])
            else:
                P.add("act", lambda e: e.activation(out=ap, in_=ap, func=AF.Sqrt), reads=[key], writes=[key])
                P.add("dve", lambda e: e.reciprocal(out=ap, in_=ap), reads=[key], writes=[key])

        for kc in range(8):
            P.add("pe", lambda e, kc=kc: e.matmul(ps[6][0:16, :], walr[:, kc, :], hT[:, kc, :],
                                                   start=(kc == 0), stop=(kc == 7)),
                  reads=[("walr",), ("hT", kc)], writes=[("ps", 6)])
        P.add("dve", lambda e: e.tensor_copy(out=alr[0:16, :], in_=ps[6][0:16, :]),
              reads=[("ps", 6)], writes=[("alr",)])
        for s in range(NSUB):
            mm_op(ps[s][:], [(alr[:, s * 128:(s + 1) * 128], wal18b[:])],
                  reads=[("alr",), ("wal18b",)], writes=[("ps", s)])
            i = next_at()
            P.add("act", lambda e, s=s, i=i: e.activation(out=at[:, i, :], in_=ps[s][:], func=AF.Exp, scale=-1.0),
                  reads=[("ps", s)], writes=[("at", i)])
            P.add("act", lambda e, s=s, i=i: e.activation(out=lv(s), in_=at[:, i, :], func=AF.Ln, bias=one_c),
                  reads=[("at", i), ("cols",)], writes=[("B", 16 + s)])
        cnt = 0
        for half in range(2):
            slot = stream(s_mw[MW[f"vb{half}"]], 4096, ("mw", f"vb{half}"))
            W = wview(slot, 8, 512)
            for s in range(NSUB):
                bank = 4 + cnt % 4
                cnt += 1
                mm_op(ps[bank][:], [(hT[:, kc, s * 128:(s + 1) * 128], W[:, kc, :]) for kc in range(8)],
                      reads=[("W", slot)] + HT_ALL, writes=[("ps", bank)])
                dst = vbm[:, s, half * 512:(half + 1) * 512]
                if cnt % 2 == 0:
                    P.add("act", lambda e, dst=dst, bank=bank: e.copy(out=dst, in_=ps[bank][:]),
                          reads=[("ps", bank)], writes=[("V", 2 * s + half)])
                else:
                    P.add("dve", lambda e, dst=dst, bank=bank: e.tensor_copy(out=dst, in_=ps[bank][:]),
                          reads=[("ps", bank)], writes=[("V", 2 * s + half)])
        slot_q = stream(s_mw[MW["q"]], 4096, ("mw", "q"))
        slot_k = stream(s_mw[MW["k"]], 4096, ("mw", "k"))
        Wq = wview(slot_q, 8, 512)
        Wk = wview(slot_k, 8, 512)
        for h in range(4):
            par = h % 2
            cb = par
            mm_multi([(ps[cb][:, s * 128:(s + 1) * 128], [(lv(s)[:, h * 128:(h + 1) * 128], U16[:])])
                      for s in range(NSUB)],
                     reads=[("B", 16 + s) for s in range(NSUB)] + [("U16",)], writes=[("ps", cb)])
            E1 = EE[:, 2 * par, :]
            E2 = EE[:, 2 * par + 1, :]
            P.add("act", lambda e, E1=E1, cb=cb: e.activation(out=E1, in_=ps[cb][:], func=AF.Exp),
                  reads=[("ps", cb)], writes=[("EE", 2 * par)])
            P.add("act", lambda e, E2=E2, cb=cb: e.activation(out=E2, in_=ps[cb][:], func=AF.Exp, scale=-1.0),
                  reads=[("ps", cb)], writes=[("EE", 2 * par + 1)])
            P.add("dve", lambda e, E1=E1, h=h: e.tensor_copy(
                out=dcol[:, 4 * h:4 * h + 4], in_=E1.rearrange("p (s n) -> p s n", s=4)[:, :, 127]),
                reads=[("EE", 2 * par)], writes=[("dcol", h)])
            mm_op(ps[2 + par][:], [(Wq[:, kc, h * 128:(h + 1) * 128], hT[:, kc, :]) for kc in range(8)],
                  reads=[("W", slot_q)] + HT_ALL, writes=[("ps", 2 + par)])
            mm_op(ps[4 + par][:], [(Wk[:, kc, h * 128:(h + 1) * 128], hT[:, kc, :]) for kc in range(8)],
                  reads=[("W", slot_k)] + HT_ALL, writes=[("ps", 4 + par)])
            P.add("dve", lambda e, E1=E1, h=h, par=par: e.scalar_tensor_tensor(
                out=qo(h), in0=ps[2 + par][:], scalar=QSCALE, in1=E1, op0=ALU.mult, op1=ALU.mult),
                reads=[("ps", 2 + par), ("EE", 2 * par)], writes=[("B", 8 + h)])
            P.add("dve", lambda e, E2=E2, h=h, par=par: e.tensor_tensor(
                out=kp(h), in0=ps[4 + par][:], in1=E2, op=ALU.mult),
                reads=[("ps", 4 + par), ("EE", 2 * par + 1)], writes=[("B", 12 + h)])
        slot_r = [stream(s_mw[MW[f"r{half}"]], 4096, ("mw", f"r{half}")) for half in range(2)]
        Wr = [wview(sl, 8, 512) for sl in slot_r]
        def og_transpose(s_):
            b2_ = s_ % 2
            tk_ = slice(s_ * 128, (s_ + 1) * 128)
            tr_multi([(psb(2)[:, c * 128:(c + 1) * 128], og_v(b2_)[:, c * 128:(c + 1) * 128], ident_b[:])
                      for c in range(8)],
                     reads=[("EE", b2_), ("ident_b",)], writes=[("ps", 2)])

            def ogt(e):
                ins = None
                for c in range(8):
                    ins = e.activation(out=ogT[:, c, tk_], in_=psb(2)[:, c * 128:(c + 1) * 128], func=AF.Copy,
                                       scale=hgh[:, c:c + 1])
                return ins
            P.add("act", ogt, reads=[("ps", 2), ("hgh",)], writes=[("ogT", c) for c in range(8)])

        for s in range(NSUB):
            b2 = s % 2
            tk = slice(s * 128, (s + 1) * 128)
            for half in range(2):
                bank = half
                mm_op(ps[bank][:], [(hT[:, kc, tk], Wr[half][:, kc, :]) for kc in range(8)],
                      reads=[("W", slot_r[half])] + HT_ALL, writes=[("ps", bank)])
                i = next_at()
                P.add("act", lambda e, i=i, bank=bank: e.activation(out=at[:, i, :], in_=ps[bank][:],
                                                                     func=AF.Tanh, scale=0.5),
                      reads=[("ps", bank)], writes=[("at", i)])
                dst = G[:, b2, half * 512:(half + 1) * 512]
                P.add("dve", lambda e, i=i, bank=bank, dst=dst: e.scalar_tensor_tensor(
                    out=dst, in0=at[:, i, :], scalar=1.0, in1=ps[bank][:], op0=ALU.add, op1=ALU.mult),
                    reads=[("at", i), ("ps", bank)], writes=[("G", b2, half)])
            mm_multi([(ps[2][:, h * 128:(h + 1) * 128], [(kp(h)[:, tk], qo(h)[:, tk])]) for h in range(4)],
                     reads=[("B", 12 + h) for h in range(4)] + [("B", 8 + h) for h in range(4)],
                     writes=[("ps", 2)])
            P.add("dve", lambda e, b2=b2: e.tensor_tensor(
                out=scm[:, b2, :], in0=ps[2][:], in1=mask4[:].rearrange("p h n -> p (h n)"), op=ALU.mult),
                reads=[("ps", 2), ("mask4",)], writes=[("scm", b2)])
            tr_multi([(psb(3)[:, h * 128:(h + 1) * 128], kp(h)[:, tk], ident_b[:]) for h in range(4)],
                     reads=[("B", 12 + h) for h in range(4)] + [("ident_b",)], writes=[("ps", 3)])
            P.add("act", lambda e, b2=b2: e.copy(out=kptm[:, b2, :], in_=psb(3)[:, 0:512]),
                  reads=[("ps", 3)], writes=[("kptm", b2)])
            items = []
            for h in range(4):
                o_ap = ps[4 + h // 2][:, (h % 2) * 256:(h % 2) * 256 + 256]
                items.append((o_ap, [(scm[:, b2, h * 128:(h + 1) * 128], vbm[:, s, h * 256:(h + 1) * 256]),
                                     (qo(h)[:, tk], Sbf[:, h, :])]))
            mm_multi(items, reads=[("scm", b2), ("V", 2 * s), ("V", 2 * s + 1), ("Sbf",)] +
                     [("B", 8 + h) for h in range(4)], writes=[("ps", 4), ("ps", 5)])
            mm_multi([(ps[6 + h // 2][:, (h % 2) * 256:(h % 2) * 256 + 256],
                       [(kptm[:, b2, h * 128:(h + 1) * 128], vbm[:, s, h * 256:(h + 1) * 256])])
                      for h in range(4)],
                     reads=[("kptm", b2), ("V", 2 * s), ("V", 2 * s + 1)], writes=[("ps", 6), ("ps", 7)])

            Sf = S[:].rearrange("p h n -> p (h n)")

            def stf1(e, Sf=Sf):
                ins = None
                for hp in range(2):
                    ins = e.tensor_tensor(out=Sf[:, hp * 512:(hp + 1) * 512], in0=ps[6 + hp][:],
                                          in1=Sf[:, hp * 512:(hp + 1) * 512], op=ALU.add)
                return ins
            P.add("dve", stf1, reads=[("ps", 6), ("ps", 7), ("S",)], writes=[("S",)])
            if s > 0:
                og_transpose(s - 1)

            def st1(e):
                ins = None
                for h in range(4):
                    o_ap = ps[4 + h // 2][:, (h % 2) * 256:(h % 2) * 256 + 256]
                    ins = e.bn_stats(out=bnst4[:, 6 * h:6 * h + 6], in_=o_ap)
                return ins
            P.add("dve", st1, reads=[("ps", 4), ("ps", 5)], writes=[("bnst4",)])

            def st2(e):
                ins = None
                for h in range(4):
                    ins = e.bn_aggr(out=mv4[:, 2 * h:2 * h + 2], in_=bnst4[:, 6 * h:6 * h + 6])
                return ins
            P.add("dve", st2, reads=[("bnst4",)], writes=[("mv4",)])
            m_ = mv4.rearrange("p (h t) -> p h t", t=2)
            P.add("dve", lambda e, m_=m_: e.tensor_tensor(out=msq, in0=m_[:, :, 0], in1=m_[:, :, 0], op=ALU.mult),
                  reads=[("mv4",)], writes=[("msq",)])
            P.add("dve", lambda e, m_=m_: e.scalar_tensor_tensor(out=rsto, in0=msq, scalar=EPS, in1=m_[:, :, 1],
                                                                 op0=ALU.add, op1=ALU.add),
                  reads=[("msq",), ("mv4",)], writes=[("rsto",)])
            rsqrt_small(rsto, 4, ("rsto",))

            def stf2(e, s=s):
                ins = None
                for h in range(4):
                    ins = e.tensor_scalar(out=S[:, h, :], in0=S[:, h, :],
                                          scalar1=dcol[:, 4 * h + s:4 * h + s + 1], scalar2=None, op0=ALU.mult)
                return ins
            def stf2p(e, s=s):
                ins = None
                for h in range(4):
                    ins = e.tensor_scalar(out=S[:, h, :], in0=S[:, h, :],
                                          scalar1=dcol[:, 4 * h + s:4 * h + s + 1], scalar2=1.0,
                                          op0=ALU.mult, op1=ALU.mult)
                return ins
            if use_pool:
                P.add("pool", stf2p, reads=[("S",)] + [("dcol", h) for h in range(4)], writes=[("S",)])
            else:
                P.add("dve", stf2, reads=[("S",)] + [("dcol", h) for h in range(4)], writes=[("S",)])
            P.add("act", lambda e, Sf=Sf: e.copy(out=Sbf[:].rearrange("p h n -> p (h n)"), in_=Sf),
                  reads=[("S",)], writes=[("Sbf",)])

            def ogf(e, b2=b2):
                ins = None
                for h in range(4):
                    o_ap = ps[4 + h // 2][:, (h % 2) * 256:(h % 2) * 256 + 256]
                    ins = e.scalar_tensor_tensor(out=og_v(b2)[:, h * 256:(h + 1) * 256], in0=o_ap,
                                                 scalar=rsto[:, h:h + 1], in1=G[:, b2, h * 256:(h + 1) * 256],
                                                 op0=ALU.mult, op1=ALU.mult)
                return ins
            P.add("dve", ogf, reads=[("ps", 4), ("ps", 5), ("rsto",), ("G", b2, 0), ("G", b2, 1)],
                  writes=[("EE", b2)])
        og_transpose(NSUB - 1)
        cnt = 0
        for g in range(2):
            slot = stream(s_mw[MW[f"u{g}"]], 4096, ("mw", f"u{g}"))
            W = wview(slot, 8, 512)
            for mi in range(4):
                m = 4 * g + mi
                bank = cnt % 4
                cnt += 1
                mm_op(ps[bank][:], [(W[:, kc, mi * 128:(mi + 1) * 128], hT[:, kc, :]) for kc in range(8)],
                      reads=[("W", slot)] + HT_ALL, writes=[("ps", bank)])
                P.add("act", lambda e, m=m, bank=bank: e.activation(out=uT(m), in_=ps[bank][:], func=AF.Gelu),
                      reads=[("ps", bank)], writes=[("B", m)])
        slot_va = [stream(s_mw[MW[f"va{half}"]], 4096, ("mw", f"va{half}")) for half in range(2)]
        Wva = [wview(sl, 8, 512) for sl in slot_va]
        scnt = [0]

        def va_proj(s):
            b2 = s % 2
            tk = slice(s * 128, (s + 1) * 128)
            for half in range(2):
                bank = 4 + 2 * b2 + half
                mm_op(ps[bank][:], [(hT[:, kc, tk], Wva[half][:, kc, :]) for kc in range(8)],
                      reads=[("W", slot_va[half])] + HT_ALL, writes=[("ps", bank)])
                dst = G[:, b2, half * 512:(half + 1) * 512]
                P.add("act", lambda e, dst=dst, bank=bank: e.activation(out=dst, in_=ps[bank][:], func=AF.Gelu),
                      reads=[("ps", bank)], writes=[("G", b2, half)])

        def va_ln(s):
            b2 = s % 2

            def ln1(e, b2=b2):
                ins = None
                for half in range(2):
                    ins = e.bn_stats(out=bnst[:, 6 * half:6 * half + 6], in_=G[:, b2, half * 512:(half + 1) * 512])
                return ins
            P.add("dve", ln1, reads=[("G", b2, 0), ("G", b2, 1)], writes=[("bnst",)])
            P.add("dve", lambda e: e.bn_aggr(out=mv, in_=bnst), reads=[("bnst",)], writes=[("mv",)])
            P.add("dve", lambda e: e.tensor_scalar(out=rln, in0=mv[:, 1:2], scalar1=EPS, scalar2=None, op0=ALU.add),
                  reads=[("mv",)], writes=[("rln",)])
            rsqrt_small(rln, 1, ("rln",))
            P.add("dve", lambda e, b2=b2: e.scalar_tensor_tensor(
                out=G[:, b2, :], in0=G[:, b2, :], scalar=mv[:, 0:1], in1=gbc[:, 0, :],
                op0=ALU.subtract, op1=ALU.mult),
                reads=[("G", b2, 0), ("G", b2, 1), ("mv",), ("gbc",)], writes=[("G", b2, 0), ("G", b2, 1)])
            P.add("dve", lambda e, b2=b2: e.scalar_tensor_tensor(
                out=vn[:, b2, :], in0=G[:, b2, :], scalar=rln, in1=gbc[:, 1, :], op0=ALU.mult, op1=ALU.add),
                reads=[("G", b2, 0), ("G", b2, 1), ("rln",), ("gbc",)], writes=[("vn", b2)])

        def va_spatial(s):
            b2 = s % 2
            tk = slice(s * 128, (s + 1) * 128)
            for gg in range(2):
                bank = scnt[0] % 4
                scnt[0] += 1
                items = []
                for g4 in range(4):
                    g = 4 * gg + g4
                    items.append((ps[bank][:, g4 * 128:(g4 + 1) * 128],
                                  [(vn[:, b2, g * 128:(g + 1) * 128], WsTm[:, g, :]),
                                   (ones_b[0:2, :], bs2hl[:, g * 128:(g + 1) * 128])]))
                mm_multi(items, reads=[("vn", b2), ("WsTm",), ("ones",), ("bs2hl",)], writes=[("ps", bank)])
                dst = Bt[:, 4 * gg:4 * gg + 4, tk]
                P.add("dve", lambda e, dst=dst, bank=bank: e.tensor_tensor(
                    out=dst, in0=ps[bank][:].rearrange("p (g n) -> p g n", g=4), in1=dst, op=ALU.mult),
                    reads=[("ps", bank)] + [("B", 4 * gg + q) for q in range(4)],
                    writes=[("B", 4 * gg + q) for q in range(4)])

        va_proj(0)
        va_proj(1)
        va_ln(0)
        va_spatial(0)
        va_proj(2)
        va_ln(1)
        va_spatial(1)
        va_proj(3)
        va_ln(2)
        va_spatial(2)
        va_ln(3)
        va_spatial(3)
        OA_ALL = [("B", c) for c in range(8)]
        OG_ALL = [("ogT", c) for c in range(8)]
        cnt = 0
        for j in range(4):
            sg = stream(s_mw[MW[f"g{j}"]], 4096, ("mw", f"g{j}"))
            sp_ = stream(s_mw[MW[f"p{j}"]], 4096, ("mw", f"p{j}"))
            Wg = wview(sg, 8, 512)
            Wp = wview(sp_, 8, 512)
            for jj in range(2):
                m = 2 * j + jj
                base = 4 * (cnt % 2)
                tb = cnt % 2
                cnt += 1
                i1 = next_at()
                i2 = next_at()
                mm_op(ps[base][:], [(Wg[:, kc, jj * 128:(jj + 1) * 128], hT[:, kc, :]) for kc in range(8)],
                      reads=[("W", sg)] + HT_ALL, writes=[("ps", base)])
                P.add("act", lambda e, i1=i1, base=base, m=m: e.activation(
                    out=at[:, i1, :], in_=ps[base][:], func=AF.Tanh, bias=bgh[:, m:m + 1], scale=0.5),
                    reads=[("ps", base), ("bgh",)], writes=[("at", i1)])
                mm_op(ps[base + 1][:], [(Wp[:, kc, jj * 128:(jj + 1) * 128], uT(kc)) for kc in range(8)],
                      reads=[("W", sp_)] + OA_ALL, writes=[("ps", base + 1)])
                P.add("dve", lambda e, i1=i1, base=base, tb=tb: e.scalar_tensor_tensor(
                    out=t1_v(tb), in0=at[:, i1, :], scalar=1.0, in1=ps[base + 1][:], op0=ALU.add, op1=ALU.mult),
                    reads=[("at", i1), ("ps", base + 1)], writes=[("EE", 2 + tb)])
                mm_op(ps[base + 2][:], [(Wg[:, kc, (2 + jj) * 128:(3 + jj) * 128], hT[:, kc, :]) for kc in range(8)],
                      reads=[("W", sg)] + HT_ALL, writes=[("ps", base + 2)])
                P.add("act", lambda e, i2=i2, base=base, m=m: e.activation(
                    out=at[:, i2, :], in_=ps[base + 2][:], func=AF.Tanh, bias=bgh[:, 8 + m:9 + m], scale=0.5),
                    reads=[("ps", base + 2), ("bgh",)], writes=[("at", i2)])
                mm_op(ps[base + 3][:], [(Wp[:, kc, (2 + jj) * 128:(3 + jj) * 128], ogT[:, kc, :]) for kc in range(8)],
                      reads=[("W", sp_)] + OG_ALL, writes=[("ps", base + 3)])

                P.add("dve", lambda e, i2=i2, base=base: e.scalar_tensor_tensor(
                    out=at[:, i2, :], in0=at[:, i2, :], scalar=1.0, in1=ps[base + 3][:], op0=ALU.add, op1=ALU.mult),
                    reads=[("at", i2), ("ps", base + 3)], writes=[("at", i2)])
                P.add("dve", lambda e, i2=i2, tb=tb, m=m: e.tensor_tensor(
                    out=mT(m), in0=t1_v(tb), in1=at[:, i2, :], op=ALU.add),
                    reads=[("at", i2), ("EE", 2 + tb)], writes=[("V", m)])
        MT_ALL = [("V", c) for c in range(8)]
        cnt = 0
        for g in range(2):
            slot = stream(s_mw[MW[f"wo{g}"]], 4096, ("mw", f"wo{g}"))
            W = wview(slot, 8, 512)
            for mi in range(4):
                m = 4 * g + mi
                bank = cnt % 4
                cnt += 1
                mm_op(ps[bank][:], [(W[:, kc, mi * 128:(mi + 1) * 128], mT(kc)) for kc in range(8)],
                      reads=[("W", slot)] + MT_ALL, writes=[("ps", bank)])
                if m > 0:
                    norm_mm(m - 1)
                P.add("dve", lambda e, m=m, bank=bank: e.scalar_tensor_tensor(
                    out=xT[:, m, :], in0=ps[bank][:], scalar=0.5, in1=xT[:, m, :], op0=ALU.mult, op1=ALU.add),
                    reads=[("ps", bank), ("xT", m)], writes=[("xT", m)])
                norm_sq(m)
        norm_mm(7)

    def output(p):
        cnt = 0
        for s in range(NSUB):
            tk = slice(s * 128, (s + 1) * 128)
            for half in range(2):
                bank = cnt % 4
                cnt += 1
                tr_multi([(ps[bank][:, j * 128:(j + 1) * 128], xT[:, 4 * half + j, tk], ident_f[:])
                          for j in range(4)],
                         reads=[("xT", 4 * half + j) for j in range(4)] + [("ident_f",)], writes=[("ps", bank)])
                b = state["ost"] % 2
                state["ost"] += 1
                if cnt % 2 == 0:
                    P.add("act", lambda e, b=b, bank=bank: e.copy(out=ost[:, b, :], in_=ps[bank][:]),
                          reads=[("ps", bank)], writes=[("ost", b)])
                else:
                    P.add("dve", lambda e, b=b, bank=bank: e.tensor_copy(out=ost[:, b, :], in_=ps[bank][:]),
                          reads=[("ps", bank)], writes=[("ost", b)])
                t0 = p * T + s * 128
                dst = y_d[t0:t0 + 128, half * 512:(half + 1) * 512]
                P.add("pool", lambda e, b=b, dst=dst: e.dma_start(out=dst, in_=ost[:, b, :]),
                      reads=[("ost", b)], writes=[("ydram", b)], dma_sem=f"ost{b}")

    setup()
    pend = [issue_xload(0, 0), issue_xload(0, 1)]
    for p in range(n_pass):
        bufs = list(pend)
        for s in range(NSUB):
            if s >= 2:
                bufs.append(issue_xload(p, s))
            x_transpose(s, bufs[s])
        if p + 1 < n_pass:
            pend = [issue_xload(p + 1, 0), issue_xload(p + 1, 1)]
        for c in range(8):
            norm_sq(c)
            if c > 0:
                norm_mm(c - 1)
        norm_mm(7)
        norm_finish(CN_F1)
        ffn(s_f1i, s_f1o, "f1i", "f1o")
        norm_finish(CN_MX)
        mixer(p)
        norm_finish(CN_F2)
        ffn(s_f2i, s_f2o, "f2i", "f2o")
        norm_finish(CN_FIN, final=True)
        output(p)

    P.emit(nc, es, final_wait_sems=("ost0", "ost1"))
    es.close()
    return nc


def _colmat(v):
    v = np.asarray(v, np.float32).reshape(-1, 128)
    return v.T


def make_in_maps(inputs, n_tok=SEQ, n_cores=8):
    f = lambda k: np.ascontiguousarray(np.asarray(inputs[k], np.float32))
    cols = np.zeros((128, 64), np.float32)
    cols[:, 0:8] = _colmat(f("ffn1_norm")[0])
    cols[:, 8:16] = _colmat(f("mix_norm")[0])
    cols[:, 16:24] = _colmat(f("ffn2_norm")[0])
    cols[:, 24:32] = _colmat(f("final_norm"))
    cols[:, 32:48] = _colmat(f("b_gate")[0])
    cols[:, 48:56] = _colmat(f("b_head_norm")[0].reshape(-1))
    pidx = np.arange(128)
    cols[:, 56] = (pidx < 17)
    cols[:, 57] = (pidx == 17)
    cols[:, 58] = (pidx == 0)
    cols[:, 59] = (pidx == 1)
    cols[:, 60] = EPS
    cols[:, 61] = 1.0
    rows = np.empty((128, 2048), np.float32)
    rows[:, 0:1024] = f("a_ln_gain")[0][None, :]
    rows[:, 1024:2048] = f("a_ln_bias")[0][None, :]
    wst = np.ascontiguousarray(f("a_w_s")[0].transpose(2, 0, 1)).reshape(128, 1024)
    bs2 = np.ascontiguousarray(np.broadcast_to(f("a_b_s")[0].reshape(1, 1024), (2, 1024)))
    wal = np.empty((18, 512), np.float32)
    wal[0:16] = f("b_w_alpha")[0]
    wal[16] = f("b_b_alpha")[0]
    wal[17] = f("b_b_alpha")[0]
    ident = np.eye(128, dtype=np.float32)
    umask = np.triu(np.ones((128, 128), np.float32))
    shared = {
        "ffn1_w_in": f("ffn1_w_in")[0], "ffn1_w_out": f("ffn1_w_out")[0], "w_in": f("w_in")[0],
        "w_proj_a": f("w_proj_a")[0], "w_proj_b": f("w_proj_b")[0], "w_o": f("w_o")[0],
        "ffn2_w_in": f("ffn2_w_in")[0], "ffn2_w_out": f("ffn2_w_out")[0],
        "cols": cols, "rows_bc": rows, "wst": wst, "bs2": bs2, "wal18": wal, "ident": ident, "umask": umask,
    }
    x = f("x")
    maps = []
    for c in range(n_cores):
        m = dict(shared)
        m["x"] = np.ascontiguousarray(x[c, :n_tok, :])
        maps.append(m)
    return maps


_NC_CACHE = {}


def kernel(**inputs):
    n_tok = SEQ
    if n_tok not in _NC_CACHE:
        _NC_CACHE[n_tok] = build_nc(n_tok)
    nc = _NC_CACHE[n_tok]
    in_maps = make_in_maps(inputs, n_tok, 8)
    res = run_bass_kernel_spmd(nc, in_maps, core_ids=list(range(8)))
    out = np.stack([np.asarray(r["y"], np.float32) for r in res.results], axis=0)
    return out
```

```python
import numpy as np
from contextlib import ExitStack
import concourse.bass as bass
import concourse.mybir as mybir
from concourse.bass_utils import run_bass_kernel_spmd

F32 = mybir.dt.float32
BF16 = mybir.dt.bfloat16
AF = mybir.ActivationFunctionType
ALU = mybir.AluOpType

D = 1024
DFF = 2816
NF = DFF // 128
DIN = 7184
T = 512
NSUB = T // 128
SEQ = 8192
EPS = 1e-6
EPOCH = 30000
QSCALE = 128 ** -0.5

C_U, C_VA, C_Q, C_K, C_VB, C_R, C_ALR, C_G = 0, 1024, 2048, 2560, 3072, 4096, 5120, 5136


class Op:
    __slots__ = ("eng", "fn", "deps", "idx", "signal", "sigidx", "dma", "sem", "semval")


class Prog:
    ENGS = ("pe", "act", "dve", "pool", "sp")

    def __init__(self):
        self.ops = []
        self.lw = {}
        self.rd = {}
        self.dma_cnt = {}

    def add(self, eng, fn, reads=(), writes=(), dma_sem=None):
        i = len(self.ops)
        deps = set()
        for k in reads:
            w = self.lw.get(k)
            if w is not None:
                deps.add(w)
        for k in writes:
            w = self.lw.get(k)
            if w is not None:
                deps.add(w)
            r = self.rd.get(k)
            if r:
                deps.update(r)
        for k in writes:
            self.lw[k] = i
            self.rd[k] = []
        for k in reads:
            if k in self.rd:
                self.rd[k].append(i)
            else:
                self.rd[k] = [i]
        deps.discard(i)
        op = Op()
        op.eng, op.fn, op.deps, op.idx = eng, fn, deps, i
        op.signal, op.sigidx = False, -1
        op.dma = dma_sem is not None
        op.sem, op.semval = None, 0
        if op.dma:
            c = self.dma_cnt.get(dma_sem, 0) + 1
            self.dma_cnt[dma_sem] = c
            op.sem, op.semval = dma_sem, 16 * c
        self.ops.append(op)
        return i

    def finalize(self):
        ops = self.ops
        for op in ops:
            keep = set()
            for d in op.deps:
                o = ops[d]
                if o.eng == op.eng and (op.eng == "pe" or (o.dma and op.dma)):
                    continue
                keep.add(d)
            op.deps = keep
            for d in keep:
                if not ops[d].dma:
                    ops[d].signal = True
        cnt = {e: 0 for e in self.ENGS}
        for op in ops:
            if op.signal:
                op.sigidx = cnt[op.eng]
                cnt[op.eng] += 1
        self.sig_cnt = cnt

    def emit(self, nc, es, final_wait_sems=()):
        self.finalize()
        ops = self.ops
        eng_sems = {}
        for e in self.ENGS:
            n = self.sig_cnt[e] // EPOCH + 1
            eng_sems[e] = [es.enter_context(nc.semaphore(f"e_{e}_{k}")) for k in range(n)]
        dma_sems = {name: es.enter_context(nc.semaphore(f"d_{name}")) for name in self.dma_cnt}
        per_eng = {e: [op for op in ops if op.eng == e] for e in self.ENGS}
        block = es.enter_context(nc.Block())

        def run(engname, e):
            waited = {}
            for op in per_eng[engname]:
                need = {}
                for d in op.deps:
                    o = ops[d]
                    if o.dma:
                        key = ("d", o.sem)
                        val = o.semval
                        sem = dma_sems[o.sem]
                    else:
                        ep = o.sigidx // EPOCH
                        key = ("e", o.eng, ep)
                        val = o.sigidx % EPOCH + 1
                        sem = eng_sems[o.eng][ep]
                    if val > need.get(key, (0, None))[0]:
                        need[key] = (val, sem)
                for key, (val, sem) in need.items():
                    if waited.get(key, 0) >= val:
                        continue
                    waited[key] = val
                    e.wait_ge(sem, val)
                ins = op.fn(e)
                if op.dma:
                    ins.then_inc(dma_sems[op.sem], 16)
                elif op.signal:
                    ins.then_inc(eng_sems[engname][op.sigidx // EPOCH], 1)
            if engname == "pool":
                for name in final_wait_sems:
                    if name in self.dma_cnt:
                        e.wait_ge(dma_sems[name], 16 * self.dma_cnt[name])

        @block.tensor
        def _(e):
            run("pe", e)

        @block.scalar
        def _(e):
            run("act", e)

        @block.vector
        def _(e):
            run("dve", e)

        @block.gpsimd
        def _(e):
            run("pool", e)

        @block.sync
        def _(e):
            run("sp", e)


def build_nc(n_tok=SEQ):
    assert n_tok % T == 0
    n_pass = n_tok // T
    nc = bass.Bass("TRN2", target_bir_lowering=False)
    P = Prog()
    es = ExitStack()

    def dram_in(name, shape):
        return nc.dram_tensor(name, list(shape), F32, kind="ExternalInput").ap()

    x_d = dram_in("x", (n_tok, D))
    f1i_d = dram_in("ffn1_w_in", (D, 2 * DFF))
    f1o_d = dram_in("ffn1_w_out", (DFF, D))
    win_d = dram_in("w_in", (D, DIN))
    wpa_d = dram_in("w_proj_a", (D, D))
    wpb_d = dram_in("w_proj_b", (D, D))
    wo_d = dram_in("w_o", (D, D))
    f2i_d = dram_in("ffn2_w_in", (D, 2 * DFF))
    f2o_d = dram_in("ffn2_w_out", (DFF, D))
    cols_d = dram_in("cols", (128, 64))
    rows_d = dram_in("rows_bc", (128, 2048))
    wst_d = dram_in("wst", (128, 1024))
    bs2_d = dram_in("bs2", (2, 1024))
    wal_d = dram_in("wal18", (18, 512))
    ident_d = dram_in("ident", (128, 128))
    umask_d = dram_in("umask", (128, 128))
    y_d = nc.dram_tensor("y", [n_tok, D], F32, kind="ExternalOutput").ap()

    def scr(name, g, n):
        return nc.dram_tensor(name, [g, 128, n], BF16, kind="Internal").ap()

    s_f1i = scr("s_f1i", 11, 4096)
    s_f1o = scr("s_f1o", 8, NF * 128)
    s_mw = scr("s_mw", 20, 4096)
    s_f2i = scr("s_f2i", 11, 4096)
    s_f2o = scr("s_f2o", 8, NF * 128)
    MW = {n: i for i, n in enumerate(
        ["u0", "u1", "va0", "va1", "q", "k", "vb0", "vb1", "r0", "r1",
         "g0", "g1", "g2", "g3", "p0", "p1", "p2", "p3", "wo0", "wo1"])}

    def sb(name, shape, dt):
        return es.enter_context(nc.sbuf_tensor("sb_" + name, list(shape), dt))

    xT = sb("xT", (128, 8, T), F32)
    hT = sb("hT", (128, 8, T), BF16)
    Bt = sb("Bt", (128, NF, T), BF16)
    at = sb("at", (128, 3, T), F32)
    sq = sb("sq", (128, 2, T), BF16)
    xin = sb("xin", (128, 2, D), F32)
    ost = sb("ost", (128, 2, T), F32)
    ring = sb("ring", (128, 4, 4096), BF16)
    gbc = sb("gbc", (128, 2, D), F32)
    vbm = sb("vbm", (128, 4, D), BF16)
    EE = sb("EE", (128, 4, T), F32)
    kptm = sb("kptm", (128, 2, 512), BF16)
    scm = sb("scm", (128, 2, 512), BF16)
    S = sb("S", (128, 4, 256), F32)
    Sbf = sb("Sbf", (128, 4, 256), BF16)
    Sd = sb("Sd", (128, 4, 256), F32)
    ogT = sb("ogT", (128, 8, T), BF16)
    G = sb("G", (128, 2, D), F32)
    vn = sb("vn", (128, 2, D), BF16)
    alr = sb("alr", (18, T), BF16)
    ident_f = sb("ident_f", (128, 128), F32)
    ident_b = sb("ident_b", (128, 128), BF16)
    U_f = sb("U_f", (128, 128), F32)
    U16 = sb("U16", (128, 128), BF16)
    mask4 = sb("mask4", (128, 4, 128), BF16)
    WsTm = sb("WsTm", (128, 8, 128), BF16)
    bs2hl = sb("bs2hl", (2, D), BF16)
    wal18b = sb("wal18b", (18, 512), BF16)
    walr = sb("walr", (128, 8, 16), BF16)
    ones_b = sb("ones_b", (128, 128), BF16)
    cols = sb("cols", (128, 64), F32)
    small = sb("small", (128, 128), F32)

    ps = [es.enter_context(nc.psum_tensor(f"ps{b}", [128, 512], F32)) for b in range(8)]

    CN_F1, CN_MX, CN_F2, CN_FIN, CN_BG, CN_HG = 0, 8, 16, 24, 32, 48
    eps_c = cols[:, 60:61]
    one_c = cols[:, 61:62]
    bgh = small[:, 0:16]
    hgh = small[:, 16:24]
    negh = small[:, 24:40]
    dcol = small[:, 40:56]
    bnst4 = small[:, 56:80]
    mv4 = small[:, 80:88]
    msq = small[:, 88:92]
    rsto = small[:, 92:96]
    bnst = small[:, 96:108]
    mv = small[:, 108:110]
    rln = small[:, 110:111]

    def og_v(b):
        return EE[:, b, :].bitcast(BF16)

    def t1_v(b):
        return EE[:, 2 + b, :]

    rstd_t = EE[:, 2, :]
    G4 = G[:].rearrange("p a (b n) -> p (a b) n", b=2)

    def ych(c):
        return G4[:, c, :] if c < 4 else EE[:, c - 4, :]

    def ykey(c):
        return ("G", c // 2, c % 2) if c < 4 else ("EE", c - 4)

    def uT(m):
        return Bt[:, m, :]

    def qo(h):
        return Bt[:, 8 + h, :]

    def kp(h):
        return Bt[:, 12 + h, :]

    def lv(s):
        return Bt[:, 16 + s, :]

    vbm_flat = vbm[:].rearrange("p s n -> p (s n)")

    def mT(m):
        return vbm_flat[:, m * T:(m + 1) * T]

    def psb(b):
        return ps[b][:].bitcast(BF16)

    state = {"at": 0, "wcnt": 0, "xin": 0, "ost": 0}

    def next_at():
        i = state["at"] % 3
        state["at"] += 1
        return i

    def mm_op(out_ap, pairs, reads, writes):
        pairs = list(pairs)

        def fn(e):
            n = len(pairs)
            ins = None
            for i, (l, r) in enumerate(pairs):
                ins = e.matmul(out_ap, l, r, start=(i == 0), stop=(i == n - 1))
            return ins
        P.add("pe", fn, reads, writes)

    def mm_multi(items, reads, writes):
        items = [(o, list(pp)) for o, pp in items]

        def fn(e):
            ins = None
            for o, pp in items:
                n = len(pp)
                for i, (l, r) in enumerate(pp):
                    ins = e.matmul(o, l, r, start=(i == 0), stop=(i == n - 1))
            return ins
        P.add("pe", fn, reads, writes)

    def tr_multi(items, reads, writes):
        items = list(items)

        def fn(e):
            ins = None
            for o, i_, idn in items:
                ins = e.transpose(o, i_, idn)
            return ins
        P.add("pe", fn, reads, writes)

    def stream(src_ap, n, scr_key):
        slot = state["wcnt"] % 4
        state["wcnt"] += 1
        dst = ring[:, slot, 0:n]
        P.add("sp", lambda e: e.dma_start(out=dst, in_=src_ap), reads=[("scr", scr_key)],
              writes=[("W", slot)], dma_sem=f"w{slot}")
        return slot

    def wview(slot, k, n):
        return ring[:, slot, 0:k * n].rearrange("p (k n) -> p k n", k=k)

    HT_ALL = [("hT", c) for c in range(8)]

    def setup():
        cl = "c"

        def ld(dst, src, key):
            P.add("sp", lambda e: e.dma_start(out=dst, in_=src), writes=[key], dma_sem=cl)
        ld(cols[:], cols_d, ("cols",))
        ld(ident_f[:], ident_d, ("ident_f",))
        ld(U_f[:], umask_d, ("U_f",))
        ld(gbc[:].rearrange("p a n -> p (a n)"), rows_d, ("gbc",))
        wst_st = at[:].rearrange("p a n -> p (a n)")[:, 0:1024]
        P.add("sp", lambda e: e.dma_start(out=wst_st, in_=wst_d),
              writes=[("at", 0), ("at", 1)], dma_sem=cl)
        bs2_st = G[0:2, 0, :]
        P.add("sp", lambda e: e.dma_start(out=bs2_st, in_=bs2_d), writes=[("G", 0, 0), ("G", 0, 1)], dma_sem=cl)
        wal_st = G[0:18, 1, 0:512]
        P.add("sp", lambda e: e.dma_start(out=wal_st, in_=wal_d), writes=[("G", 1, 0)], dma_sem=cl)
        fence_keys = [("cols",), ("ident_f",), ("U_f",), ("gbc",), ("at", 0), ("at", 1), ("G", 0, 0), ("G", 0, 1), ("G", 1, 0)]
        P.add("sp", lambda e: e.dma_start(out=small[0:1, 112:128], in_=cols_d[0:1, 0:16]),
              reads=[], writes=fence_keys + [("fence",)], dma_sem=cl)

        P.add("pool", lambda e: e.dma_start(
            out=walr[:], in_=win_d[:, C_ALR:C_ALR + 16].rearrange("(k p) n -> p k n", p=128)),
            writes=[("walr",)], dma_sem="walr")

        P.add("dve", lambda e: e.tensor_copy(out=ident_b[:], in_=ident_f[:]),
              reads=[("ident_f",)], writes=[("ident_b",)])
        P.add("dve", lambda e: e.tensor_scalar(out=U16[:], in0=U_f[:], scalar1=-1.0 / 16.0, scalar2=None,
                                               op0=ALU.mult), reads=[("U_f",)], writes=[("U16",)])

        def m4(e):
            ins = None
            for h in range(4):
                ins = e.tensor_copy(out=mask4[:, h, :], in_=U_f[:])
            return ins
        P.add("dve", m4, reads=[("U_f",)], writes=[("mask4",)])

        def wsm(e):
            ins = None
            for g in range(8):
                ins = e.tensor_tensor(out=WsTm[:, g, :], in0=wst_st[:, g * 128:(g + 1) * 128], in1=U_f[:],
                                      op=ALU.mult)
            return ins
        P.add("dve", wsm, reads=[("at", 0), ("at", 1), ("U_f",)], writes=[("WsTm",)])

        P.add("dve", lambda e: e.memset(ones_b[:], 1.0), writes=[("ones",)])
        P.add("dve", lambda e: e.memset(alr[:], 1.0), writes=[("alr",)])
        P.add("dve", lambda e: e.memset(S[:].rearrange("p h n -> p (h n)"), 0.0), writes=[("S",)])
        P.add("dve", lambda e: e.memset(Sbf[:].rearrange("p h n -> p (h n)"), 0.0), writes=[("Sbf",)])
        P.add("dve", lambda e: e.memset(negh, -0.5), writes=[("negh",)])
        P.add("dve", lambda e: e.tensor_scalar(out=bgh, in0=cols[:, CN_BG:CN_BG + 16], scalar1=0.5,
                                               scalar2=None, op0=ALU.mult),
              reads=[("cols",)], writes=[("bgh",)])
        P.add("dve", lambda e: e.tensor_scalar(out=hgh, in0=cols[:, CN_HG:CN_HG + 8], scalar1=0.5,
                                               scalar2=None, op0=ALU.mult),
              reads=[("cols",)], writes=[("hgh",)])

        def hilo(dst, st, np_, n, selA, selD, tmpA, tmpD, rk, wk):
            allk = rk + wk + [("cols",)]
            P.add("dve", lambda e: e.tensor_copy(out=dst, in_=st), reads=allk, writes=wk)
            P.add("dve", lambda e: e.tensor_tensor(out=tmpD, in0=st, in1=dst, op=ALU.subtract),
                  reads=allk, writes=wk)
            P.add("dve", lambda e: e.tensor_scalar(out=tmpD, in0=tmpD, scalar1=selD, scalar2=None, op0=ALU.mult),
                  reads=allk, writes=wk)
            P.add("dve", lambda e: e.tensor_copy(out=tmpA, in_=dst), reads=allk, writes=wk)
            P.add("dve", lambda e: e.scalar_tensor_tensor(out=dst, in0=tmpA, scalar=selA, in1=tmpD,
                                                          op0=ALU.mult, op1=ALU.add), reads=allk, writes=wk)
        hilo(bs2hl[:], bs2_st, 2, 1024, cols[0:2, 58:59], cols[0:2, 59:60],
             EE[0:2, 0:2, :].rearrange("p a n -> p (a n)"), EE[0:2, 2:4, :].rearrange("p a n -> p (a n)"),
             [("G", 0, 0), ("G", 0, 1)], [("bs2hl",), ("EE", 0), ("EE", 1), ("EE", 2), ("EE", 3)])
        hilo(wal18b[:], wal_st, 18, 512, cols[0:18, 56:57], cols[0:18, 57:58],
             EE[0:18, 0, :], EE[0:18, 2, :],
             [("G", 1, 0)], [("wal18b",), ("EE", 0), ("EE", 2)])

        def conv(dst, src, key, sem):
            P.add("pool", lambda e: e.dma_start(out=dst, in_=src), writes=[("scr", key)], dma_sem=sem)

        def conv_ffn(sin, sout, wi, wo_, key_i, key_o):
            for j in range(11):
                dv = sin[j].rearrange("p (k n) -> p k n", k=8)
                a0 = 2 * j * 128
                conv(dv[:, :, 0:256], wi[:, a0:a0 + 256].rearrange("(k p) n -> p k n", p=128),
                     (key_i, j), f"{key_i}_{j}")
                conv(dv[:, :, 256:512], wi[:, DFF + a0:DFF + a0 + 256].rearrange("(k p) n -> p k n", p=128),
                     (key_i, j), f"{key_i}_{j}")
            for m in range(8):
                dv = sout[m].rearrange("p (k n) -> p k n", k=NF)
                conv(dv, wo_[:, m * 128:(m + 1) * 128].rearrange("(k p) n -> p k n", p=128),
                     (key_o, m), f"{key_o}_{m}")

        conv_ffn(s_f1i, s_f1o, f1i_d, f1o_d, "f1i", "f1o")

        def conv_cols(name, src, c0, n=512, off=0):
            dv = s_mw[MW[name]].rearrange("p (k n) -> p k n", k=8)
            conv(dv[:, :, off:off + n], src[:, c0:c0 + n].rearrange("(k p) n -> p k n", p=128),
                 ("mw", name), f"mw_{name}")
        conv_cols("vb0", win_d, C_VB)
        conv_cols("vb1", win_d, C_VB + 512)
        conv_cols("q", win_d, C_Q)
        conv_cols("k", win_d, C_K)
        conv_cols("r0", win_d, C_R)
        conv_cols("r1", win_d, C_R + 512)
        conv_cols("u0", win_d, C_U)
        conv_cols("u1", win_d, C_U + 512)
        conv_cols("va0", win_d, C_VA)
        conv_cols("va1", win_d, C_VA + 512)
        for j in range(4):
            conv_cols(f"g{j}", win_d, C_G + 256 * j, 256, 0)
            conv_cols(f"g{j}", win_d, C_G + 1024 + 256 * j, 256, 256)
            conv_cols(f"p{j}", wpa_d, 256 * j, 256, 0)
            conv_cols(f"p{j}", wpb_d, 256 * j, 256, 256)
        conv_cols("wo0", wo_d, 0)
        conv_cols("wo1", wo_d, 512)
        conv_ffn(s_f2i, s_f2o, f2i_d, f2o_d, "f2i", "f2o")

    def issue_xload(p, s):
        b = state["xin"] % 2
        state["xin"] += 1
        t0 = p * T + s * 128
        dst = xin[:, b, :]
        src = x_d[t0:t0 + 128, :]
        P.add("sp", lambda e: e.dma_start(out=dst, in_=src), writes=[("xin", b)], dma_sem=f"xin{b}")
        return b

    def x_transpose(s, b):
        for half in range(2):
            bank = half
            tr_multi([(ps[bank][:, j * 128:(j + 1) * 128], xin[:, b, (4 * half + j) * 128:(4 * half + j + 1) * 128],
                       ident_f[:]) for j in range(4)],
                     reads=[("xin", b), ("ident_f",)], writes=[("ps", bank)])
            dst = xT[:, 4 * half:4 * half + 4, s * 128:(s + 1) * 128]
            src = ps[bank][:].rearrange("p (j n) -> p j n", j=4)
            wk = [("xT", c) for c in range(4 * half, 4 * half + 4)]
            if half == 0:
                P.add("act", lambda e, dst=dst, src=src: e.copy(out=dst, in_=src), reads=[("ps", bank)], writes=wk)
            else:
                P.add("dve", lambda e, dst=dst, src=src: e.tensor_copy(out=dst, in_=src), reads=[("ps", bank)],
                      writes=wk)

    def norm_sq(c):
        i = c % 2
        P.add("act", lambda e: e.activation(out=sq[:, i, :], in_=xT[:, c, :], func=AF.Square),
              reads=[("xT", c)], writes=[("sq", i)])

    def norm_mm(c):
        i = c % 2

        def fn(e):
            return e.matmul(ps[6][:], ones_b[:], sq[:, i, :], start=(c == 0), stop=(c == 7))
        P.add("pe", fn, reads=[("sq", i), ("ones",)], writes=[("ps", 6)])

    def norm_finish(cn, final=False):
        P.add("act", lambda e: e.activation(out=small[:, 113:114], in_=cols[:, 61:62], func=AF.Sqrt),
              reads=[("cols",)], writes=[("dummy",)])
        P.add("act", lambda e: e.activation(out=rstd_t, in_=ps[6][:], func=AF.Sqrt, bias=eps_c,
                                            scale=1.0 / D),
              reads=[("ps", 6), ("cols",)], writes=[("EE", 2)])
        P.add("dve", lambda e: e.reciprocal(out=rstd_t, in_=rstd_t), reads=[("EE", 2)], writes=[("EE", 2)])
        for c in ((0, 1, 2, 3, 4, 5, 7, 6) if final else range(8)):
            if final:
                P.add("dve", lambda e, c=c: e.scalar_tensor_tensor(
                    out=ych(c), in0=xT[:, c, :], scalar=cols[:, cn + c:cn + c + 1], in1=rstd_t,
                    op0=ALU.mult, op1=ALU.mult),
                    reads=[("xT", c), ("EE", 2), ("cols",)], writes=[ykey(c)])
            else:
                P.add("dve", lambda e, c=c: e.scalar_tensor_tensor(
                    out=hT[:, c, :], in0=xT[:, c, :], scalar=cols[:, cn + c:cn + c + 1], in1=rstd_t,
                    op0=ALU.mult, op1=ALU.mult),
                    reads=[("xT", c), ("EE", 2), ("cols",)], writes=[("hT", c)])

    def ffn(s_in, s_out, key_i, key_o):
        cnt = 0
        for j in range(11):
            slot = stream(s_in[j], 4096, (key_i, j))
            W = wview(slot, 8, 512)
            ai = {}
            order = (("a", 0), ("b", 0), ("a", 1), ("b", 1))
            if j == 0:
                for kc in range(8):
                    for q_, (kind, jj) in enumerate(order):
                        mi = jj if kind == "a" else 2 + jj
                        P.add("pe", lambda e, kc=kc, q_=q_, mi=mi, W=W: e.matmul(
                            ps[q_][:], W[:, kc, mi * 128:(mi + 1) * 128], hT[:, kc, :],
                            start=(kc == 0), stop=(kc == 7)),
                            reads=[("W", slot), ("hT", kc)], writes=[("ps", q_)])
            for (kind, jj) in order:
                mi = jj if kind == "a" else 2 + jj
                bank = cnt % 4
                cnt += 1
                if j > 0:
                    mm_op(ps[bank][:], [(W[:, kc, mi * 128:(mi + 1) * 128], hT[:, kc, :]) for kc in range(8)],
                          reads=[("W", slot)] + HT_ALL, writes=[("ps", bank)])
                if kind == "a":
                    i = next_at()
                    ai[jj] = i
                    P.add("act", lambda e, i=i, bank=bank: e.activation(out=at[:, i, :], in_=ps[bank][:],
                                                                         func=AF.Silu),
                          reads=[("ps", bank)], writes=[("at", i)])
                else:
                    i = ai[jj]
                    c = 2 * j + jj
                    P.add("dve", lambda e, i=i, bank=bank, c=c: e.tensor_tensor(
                        out=Bt[:, c, :], in0=ps[bank][:], in1=at[:, i, :], op=ALU.mult),
                        reads=[("ps", bank), ("at", i)], writes=[("B", c)])
        for m in range(8):
            slot = stream(s_out[m], NF * 128, (key_o, m))
            Wo = wview(slot, NF, 128)
            bank = 4 + m % 2
            mm_op(ps[bank][:], [(Wo[:, kc, :], Bt[:, kc, :]) for kc in range(NF)],
                  reads=[("W", slot)] + [("B", c) for c in range(NF)], writes=[("ps", bank)])
            if m > 0:
                norm_mm(m - 1)
            P.add("dve", lambda e, m=m, bank=bank: e.scalar_tensor_tensor(
                out=xT[:, m, :], in0=ps[bank][:], scalar=0.5, in1=xT[:, m, :], op0=ALU.mult, op1=ALU.add),
                reads=[("ps", bank), ("xT", m)], writes=[("xT", m)])
            norm_sq(m)
        norm_mm(7)

    def mixer(p):
        use_pool = p > 0

        def rsqrt_small(ap, n, key):
            if use_pool:
                P.add("pool", lambda e: e.tensor_tensor(out=ap, in0=ap, in1=negh[:, 0:n], op=ALU.pow),
                      reads=[key, ("negh",)], writes=[key])
            else:
                P.add("act", lambda e: e.activation(out=ap, in_=ap, func=AF.Sqrt), reads=[key], writes=[key])
                P.add("dve", lambda e: e.reciprocal(out=ap, in_=ap), reads=[key], writes=[key])

        for kc in range(8):
            P.add("pe", lambda e, kc=kc: e.matmul(ps[6][0:16, :], walr[:, kc, :], hT[:, kc, :],
                                                   start=(kc == 0), stop=(kc == 7)),
                  reads=[("walr",), ("hT", kc)], writes=[("ps", 6)])
        P.add("dve", lambda e: e.tensor_copy(out=alr[0:16, :], in_=ps[6][0:16, :]),
              reads=[("ps", 6)], writes=[("alr",)])
        for s in range(NSUB):
            mm_op(ps[s][:], [(alr[:, s * 128:(s + 1) * 128], wal18b[:])],
                  reads=[("alr",), ("wal18b",)], writes=[("ps", s)])
            i = next_at()
            P.add("act", lambda e, s=s, i=i: e.activation(out=at[:, i, :], in_=ps[s][:], func=AF.Exp, scale=-1.0),
                  reads=[("ps", s)], writes=[("at", i)])
            P.add("act", lambda e, s=s, i=i: e.activation(out=lv(s), in_=at[:, i, :], func=AF.Ln, bias=one_c),
                  reads=[("at", i), ("cols",)], writes=[("B", 16 + s)])
        cnt = 0
        for half in range(2):
            slot = stream(s_mw[MW[f"vb{half}"]], 4096, ("mw", f"vb{half}"))
            W = wview(slot, 8, 512)
            for s in range(NSUB):
                bank = 4 + cnt % 4
                cnt += 1
                mm_op(ps[bank][:], [(hT[:, kc, s * 128:(s + 1) * 128], W[:, kc, :]) for kc in range(8)],
                      reads=[("W", slot)] + HT_ALL, writes=[("ps", bank)])
                dst = vbm[:, s, half * 512:(half + 1) * 512]
                if cnt % 2 == 0:
                    P.add("act", lambda e, dst=dst, bank=bank: e.copy(out=dst, in_=ps[bank][:]),
                          reads=[("ps", bank)], writes=[("V", 2 * s + half)])
                else:
                    P.add("dve", lambda e, dst=dst, bank=bank: e.tensor_copy(out=dst, in_=ps[bank][:]),
                          reads=[("ps", bank)], writes=[("V", 2 * s + half)])
        slot_q = stream(s_mw[MW["q"]], 4096, ("mw", "q"))
        slot_k = stream(s_mw[MW["k"]], 4096, ("mw", "k"))
        Wq = wview(slot_q, 8, 512)
        Wk = wview(slot_k, 8, 512)
        for h in range(4):
            par = h % 2
            cb = par
            mm_multi([(ps[cb][:, s * 128:(s + 1) * 128], [(lv(s)[:, h * 128:(h + 1) * 128], U16[:])])
                      for s in range(NSUB)],
                     reads=[("B", 16 + s) for s in range(NSUB)] + [("U16",)], writes=[("ps", cb)])
            E1 = EE[:, 2 * par, :]
            E2 = EE[:, 2 * par + 1, :]
            P.add("act", lambda e, E1=E1, cb=cb: e.activation(out=E1, in_=ps[cb][:], func=AF.Exp),
                  reads=[("ps", cb)], writes=[("EE", 2 * par)])
            P.add("act", lambda e, E2=E2, cb=cb: e.activation(out=E2, in_=ps[cb][:], func=AF.Exp, scale=-1.0),
                  reads=[("ps", cb)], writes=[("EE", 2 * par + 1)])
            P.add("dve", lambda e, E1=E1, h=h: e.tensor_copy(
                out=dcol[:, 4 * h:4 * h + 4], in_=E1.rearrange("p (s n) -> p s n", s=4)[:, :, 127]),
                reads=[("EE", 2 * par)], writes=[("dcol", h)])
            mm_op(ps[2 + par][:], [(Wq[:, kc, h * 128:(h + 1) * 128], hT[:, kc, :]) for kc in range(8)],
                  reads=[("W", slot_q)] + HT_ALL, writes=[("ps", 2 + par)])
            mm_op(ps[4 + par][:], [(Wk[:, kc, h * 128:(h + 1) * 128], hT[:, kc, :]) for kc in range(8)],
                  reads=[("W", slot_k)] + HT_ALL, writes=[("ps", 4 + par)])
            P.add("dve", lambda e, E1=E1, h=h, par=par: e.scalar_tensor_tensor(
                out=qo(h), in0=ps[2 + par][:], scalar=QSCALE, in1=E1, op0=ALU.mult, op1=ALU.mult),
                reads=[("ps", 2 + par), ("EE", 2 * par)], writes=[("B", 8 + h)])
            P.add("dve", lambda e, E2=E2, h=h, par=par: e.tensor_tensor(
                out=kp(h), in0=ps[4 + par][:], in1=E2, op=ALU.mult),
                reads=[("ps", 4 + par), ("EE", 2 * par + 1)], writes=[("B", 12 + h)])
        slot_r = [stream(s_mw[MW[f"r{half}"]], 4096, ("mw", f"r{half}")) for half in range(2)]
        Wr = [wview(sl, 8, 512) for sl in slot_r]
        def og_transpose(s_):
            b2_ = s_ % 2
            tk_ = slice(s_ * 128, (s_ + 1) * 128)
            tr_multi([(psb(2)[:, c * 128:(c + 1) * 128], og_v(b2_)[:, c * 128:(c + 1) * 128], ident_b[:])
                      for c in range(8)],
                     reads=[("EE", b2_), ("ident_b",)], writes=[("ps", 2)])

            def ogt(e):
                ins = None
                for c in range(8):
                    ins = e.activation(out=ogT[:, c, tk_], in_=psb(2)[:, c * 128:(c + 1) * 128], func=AF.Copy,
                                       scale=hgh[:, c:c + 1])
                return ins
            P.add("act", ogt, reads=[("ps", 2), ("hgh",)], writes=[("ogT", c) for c in range(8)])

        for s in range(NSUB):
            b2 = s % 2
            tk = slice(s * 128, (s + 1) * 128)

            def sdp_pool(e, s=s):
                ins = None
                for h in range(4):
                    ins = e.tensor_scalar(out=Sd[:, h, :], in0=S[:, h, :], scalar1=dcol[:, 4 * h + s:4 * h + s + 1],
                                          scalar2=1.0, op0=ALU.mult, op1=ALU.mult)
                return ins

            def sdp_act(e, s=s):
                ins = None
                for h in range(4):
                    ins = e.activation(out=Sd[:, h, :], in_=S[:, h, :], func=AF.Copy,
                                       scale=dcol[:, 4 * h + s:4 * h + s + 1])
                return ins
            P.add("pool" if use_pool else "act", sdp_pool if use_pool else sdp_act,
                  reads=[("S",)] + [("dcol", h) for h in range(4)], writes=[("Sd",)])
            for half in range(2):
                bank = half
                mm_op(ps[bank][:], [(hT[:, kc, tk], Wr[half][:, kc, :]) for kc in range(8)],
                      reads=[("W", slot_r[half])] + HT_ALL, writes=[("ps", bank)])
                i = next_at()
                P.add("act", lambda e, i=i, bank=bank: e.activation(out=at[:, i, :], in_=ps[bank][:],
                                                                     func=AF.Tanh, scale=0.5),
                      reads=[("ps", bank)], writes=[("at", i)])
                dst = G[:, b2, half * 512:(half + 1) * 512]
                P.add("dve", lambda e, i=i, bank=bank, dst=dst: e.scalar_tensor_tensor(
                    out=dst, in0=at[:, i, :], scalar=1.0, in1=ps[bank][:], op0=ALU.add, op1=ALU.mult),
                    reads=[("at", i), ("ps", bank)], writes=[("G", b2, half)])
            mm_multi([(ps[2][:, h * 128:(h + 1) * 128], [(kp(h)[:, tk], qo(h)[:, tk])]) for h in range(4)],
                     reads=[("B", 12 + h) for h in range(4)] + [("B", 8 + h) for h in range(4)],
                     writes=[("ps", 2)])
            P.add("dve", lambda e, b2=b2: e.tensor_tensor(
                out=scm[:, b2, :], in0=ps[2][:], in1=mask4[:].rearrange("p h n -> p (h n)"), op=ALU.mult),
                reads=[("ps", 2), ("mask4",)], writes=[("scm", b2)])
            tr_multi([(psb(3)[:, h * 128:(h + 1) * 128], kp(h)[:, tk], ident_b[:]) for h in range(4)],
                     reads=[("B", 12 + h) for h in range(4)] + [("ident_b",)], writes=[("ps", 3)])
            P.add("act", lambda e, b2=b2: e.copy(out=kptm[:, b2, :], in_=psb(3)[:, 0:512]),
                  reads=[("ps", 3)], writes=[("kptm", b2)])
            items = []
            for h in range(4):
                o_ap = ps[4 + h // 2][:, (h % 2) * 256:(h % 2) * 256 + 256]
                items.append((o_ap, [(scm[:, b2, h * 128:(h + 1) * 128], vbm[:, s, h * 256:(h + 1) * 256]),
                                     (qo(h)[:, tk], Sbf[:, h, :])]))
            mm_multi(items, reads=[("scm", b2), ("V", 2 * s), ("V", 2 * s + 1), ("Sbf",)] +
                     [("B", 8 + h) for h in range(4)], writes=[("ps", 4), ("ps", 5)])
            mm_multi([(ps[6 + h // 2][:, (h % 2) * 256:(h % 2) * 256 + 256],
                       [(kptm[:, b2, h * 128:(h + 1) * 128], vbm[:, s, h * 256:(h + 1) * 256])])
                      for h in range(4)],
                     reads=[("kptm", b2), ("V", 2 * s), ("V", 2 * s + 1)], writes=[("ps", 6), ("ps", 7)])

            def stS(e, s=s):
                ins = None
                for h in range(4):
                    p_ap = ps[6 + h // 2][:, (h % 2) * 256:(h % 2) * 256 + 256]
                    ins = e.scalar_tensor_tensor(out=S[:, h, :], in0=p_ap, scalar=dcol[:, 4 * h + s:4 * h + s + 1],
                                                 in1=Sd[:, h, :], op0=ALU.mult, op1=ALU.add)
                return ins
            P.add("dve", stS, reads=[("ps", 6), ("ps", 7), ("Sd",)] + [("dcol", h) for h in range(4)],
                  writes=[("S",)])
            P.add("act", lambda e: e.copy(out=Sbf[:].rearrange("p h n -> p (h n)"),
                                          in_=S[:].rearrange("p h n -> p (h n)")),
                  reads=[("S",)], writes=[("Sbf",)])
            if s > 0:
                og_transpose(s - 1)

            def st1(e):
                ins = None
                for h in range(4):
                    o_ap = ps[4 + h // 2][:, (h % 2) * 256:(h % 2) * 256 + 256]
                    ins = e.bn_stats(out=bnst4[:, 6 * h:6 * h + 6], in_=o_ap)
                return ins
            P.add("dve", st1, reads=[("ps", 4), ("ps", 5)], writes=[("bnst4",)])

            def st2(e):
                ins = None
                for h in range(4):
                    ins = e.bn_aggr(out=mv4[:, 2 * h:2 * h + 2], in_=bnst4[:, 6 * h:6 * h + 6])
                return ins
            P.add("dve", st2, reads=[("bnst4",)], writes=[("mv4",)])
            m_ = mv4.rearrange("p (h t) -> p h t", t=2)
            P.add("dve", lambda e, m_=m_: e.tensor_tensor(out=msq, in0=m_[:, :, 0], in1=m_[:, :, 0], op=ALU.mult),
                  reads=[("mv4",)], writes=[("msq",)])
            P.add("dve", lambda e, m_=m_: e.scalar_tensor_tensor(out=rsto, in0=msq, scalar=EPS, in1=m_[:, :, 1],
                                                                 op0=ALU.add, op1=ALU.add),
                  reads=[("msq",), ("mv4",)], writes=[("rsto",)])
            rsqrt_small(rsto, 4, ("rsto",))

            def ogf(e, b2=b2):
                ins = None
                for h in range(4):
                    o_ap = ps[4 + h // 2][:, (h % 2) * 256:(h % 2) * 256 + 256]
                    ins = e.scalar_tensor_tensor(out=og_v(b2)[:, h * 256:(h + 1) * 256], in0=o_ap,
                                                 scalar=rsto[:, h:h + 1], in1=G[:, b2, h * 256:(h + 1) * 256],
                                                 op0=ALU.mult, op1=ALU.mult)
                return ins
            P.add("dve", ogf, reads=[("ps", 4), ("ps", 5), ("rsto",), ("G", b2, 0), ("G", b2, 1)],
                  writes=[("EE", b2)])
        og_transpose(NSUB - 1)
        cnt = 0
        for g in range(2):
            slot = stream(s_mw[MW[f"u{g}"]], 4096, ("mw", f"u{g}"))
            W = wview(slot, 8, 512)
            for mi in range(4):
                m = 4 * g + mi
                bank = cnt % 4
                cnt += 1
                mm_op(ps[bank][:], [(W[:, kc, mi * 128:(mi + 1) * 128], hT[:, kc, :]) for kc in range(8)],
                      reads=[("W", slot)] + HT_ALL, writes=[("ps", bank)])
                P.add("act", lambda e, m=m, bank=bank: e.activation(out=uT(m), in_=ps[bank][:], func=AF.Gelu),
                      reads=[("ps", bank)], writes=[("B", m)])
        slot_va = [stream(s_mw[MW[f"va{half}"]], 4096, ("mw", f"va{half}")) for half in range(2)]
        Wva = [wview(sl, 8, 512) for sl in slot_va]
        scnt = [0]

        def va_proj(s):
            b2 = s % 2
            tk = slice(s * 128, (s + 1) * 128)
            for half in range(2):
                bank = 4 + 2 * b2 + half
                mm_op(ps[bank][:], [(hT[:, kc, tk], Wva[half][:, kc, :]) for kc in range(8)],
                      reads=[("W", slot_va[half])] + HT_ALL, writes=[("ps", bank)])
                dst = G[:, b2, half * 512:(half + 1) * 512]
                P.add("act", lambda e, dst=dst, bank=bank: e.activation(out=dst, in_=ps[bank][:], func=AF.Gelu),
                      reads=[("ps", bank)], writes=[("G", b2, half)])

        def va_ln(s):
            b2 = s % 2

            def ln1(e, b2=b2):
                ins = None
                for half in range(2):
                    ins = e.bn_stats(out=bnst[:, 6 * half:6 * half + 6], in_=G[:, b2, half * 512:(half + 1) * 512])
                return ins
            P.add("dve", ln1, reads=[("G", b2, 0), ("G", b2, 1)], writes=[("bnst",)])
            P.add("dve", lambda e: e.bn_aggr(out=mv, in_=bnst), reads=[("bnst",)], writes=[("mv",)])
            P.add("dve", lambda e: e.tensor_scalar(out=rln, in0=mv[:, 1:2], scalar1=EPS, scalar2=None, op0=ALU.add),
                  reads=[("mv",)], writes=[("rln",)])
            rsqrt_small(rln, 1, ("rln",))
            P.add("dve", lambda e, b2=b2: e.scalar_tensor_tensor(
                out=G[:, b2, :], in0=G[:, b2, :], scalar=mv[:, 0:1], in1=gbc[:, 0, :],
                op0=ALU.subtract, op1=ALU.mult),
                reads=[("G", b2, 0), ("G", b2, 1), ("mv",), ("gbc",)], writes=[("G", b2, 0), ("G", b2, 1)])
            P.add("dve", lambda e, b2=b2: e.scalar_tensor_tensor(
                out=vn[:, b2, :], in0=G[:, b2, :], scalar=rln, in1=gbc[:, 1, :], op0=ALU.mult, op1=ALU.add),
                reads=[("G", b2, 0), ("G", b2, 1), ("rln",), ("gbc",)], writes=[("vn", b2)])

        def va_spatial(s):
            b2 = s % 2
            tk = slice(s * 128, (s + 1) * 128)
            for gg in range(2):
                bank = scnt[0] % 4
                scnt[0] += 1
                items = []
                for g4 in range(4):
                    g = 4 * gg + g4
                    items.append((ps[bank][:, g4 * 128:(g4 + 1) * 128],
                                  [(vn[:, b2, g * 128:(g + 1) * 128], WsTm[:, g, :]),
                                   (ones_b[0:2, :], bs2hl[:, g * 128:(g + 1) * 128])]))
                mm_multi(items, reads=[("vn", b2), ("WsTm",), ("ones",), ("bs2hl",)], writes=[("ps", bank)])
                dst = Bt[:, 4 * gg:4 * gg + 4, tk]
                P.add("dve", lambda e, dst=dst, bank=bank: e.tensor_tensor(
                    out=dst, in0=ps[bank][:].rearrange("p (g n) -> p g n", g=4), in1=dst, op=ALU.mult),
                    reads=[("ps", bank)] + [("B", 4 * gg + q) for q in range(4)],
                    writes=[("B", 4 * gg + q) for q in range(4)])

        va_proj(0)
        va_proj(1)
        va_ln(0)
        va_spatial(0)
        va_proj(2)
        va_ln(1)
        va_spatial(1)
        va_proj(3)
        va_ln(2)
        va_spatial(2)
        va_ln(3)
        va_spatial(3)
        OA_ALL = [("B", c) for c in range(8)]
        OG_ALL = [("ogT", c) for c in range(8)]
        cnt = 0
        for j in range(4):
            sg = stream(s_mw[MW[f"g{j}"]], 4096, ("mw", f"g{j}"))
            sp_ = stream(s_mw[MW[f"p{j}"]], 4096, ("mw", f"p{j}"))
            Wg = wview(sg, 8, 512)
            Wp = wview(sp_, 8, 512)
            for jj in range(2):
                m = 2 * j + jj
                base = 4 * (cnt % 2)
                tb = cnt % 2
                cnt += 1
                i1 = next_at()
                i2 = next_at()
                mm_op(ps[base][:], [(Wg[:, kc, jj * 128:(jj + 1) * 128], hT[:, kc, :]) for kc in range(8)],
                      reads=[("W", sg)] + HT_ALL, writes=[("ps", base)])
                P.add("act", lambda e, i1=i1, base=base, m=m: e.activation(
                    out=at[:, i1, :], in_=ps[base][:], func=AF.Tanh, bias=bgh[:, m:m + 1], scale=0.5),
                    reads=[("ps", base), ("bgh",)], writes=[("at", i1)])
                mm_op(ps[base + 1][:], [(Wp[:, kc, jj * 128:(jj + 1) * 128], uT(kc)) for kc in range(8)],
                      reads=[("W", sp_)] + OA_ALL, writes=[("ps", base + 1)])
                P.add("dve", lambda e, i1=i1, base=base, tb=tb: e.scalar_tensor_tensor(
                    out=t1_v(tb), in0=at[:, i1, :], scalar=1.0, in1=ps[base + 1][:], op0=ALU.add, op1=ALU.mult),
                    reads=[("at", i1), ("ps", base + 1)], writes=[("EE", 2 + tb)])
                mm_op(ps[base + 2][:], [(Wg[:, kc, (2 + jj) * 128:(3 + jj) * 128], hT[:, kc, :]) for kc in range(8)],
                      reads=[("W", sg)] + HT_ALL, writes=[("ps", base + 2)])
                P.add("act", lambda e, i2=i2, base=base, m=m: e.activation(
                    out=at[:, i2, :], in_=ps[base + 2][:], func=AF.Tanh, bias=bgh[:, 8 + m:9 + m], scale=0.5),
                    reads=[("ps", base + 2), ("bgh",)], writes=[("at", i2)])
                mm_op(ps[base + 3][:], [(Wp[:, kc, (2 + jj) * 128:(3 + jj) * 128], ogT[:, kc, :]) for kc in range(8)],
                      reads=[("W", sp_)] + OG_ALL, writes=[("ps", base + 3)])

                P.add("dve", lambda e, i2=i2, base=base: e.scalar_tensor_tensor(
                    out=at[:, i2, :], in0=at[:, i2, :], scalar=1.0, in1=ps[base + 3][:], op0=ALU.add, op1=ALU.mult),
                    reads=[("at", i2), ("ps", base + 3)], writes=[("at", i2)])
                P.add("dve", lambda e, i2=i2, tb=tb, m=m: e.tensor_tensor(
                    out=mT(m), in0=t1_v(tb), in1=at[:, i2, :], op=ALU.add),
                    reads=[("at", i2), ("EE", 2 + tb)], writes=[("V", m)])
        MT_ALL = [("V", c) for c in range(8)]
        cnt = 0
        for g in range(2):
            slot = stream(s_mw[MW[f"wo{g}"]], 4096, ("mw", f"wo{g}"))
            W = wview(slot, 8, 512)
            for mi in range(4):
                m = 4 * g + mi
                bank = cnt % 4
                cnt += 1
                mm_op(ps[bank][:], [(W[:, kc, mi * 128:(mi + 1) * 128], mT(kc)) for kc in range(8)],
                      reads=[("W", slot)] + MT_ALL, writes=[("ps", bank)])
                if m > 0:
                    norm_mm(m - 1)
                P.add("dve", lambda e, m=m, bank=bank: e.scalar_tensor_tensor(
                    out=xT[:, m, :], in0=ps[bank][:], scalar=0.5, in1=xT[:, m, :], op0=ALU.mult, op1=ALU.add),
                    reads=[("ps", bank), ("xT", m)], writes=[("xT", m)])
                norm_sq(m)
        norm_mm(7)

    ocnt = [0]

    def output(p, half):
        for s in range(NSUB):
            tk = slice(s * 128, (s + 1) * 128)
            bank = 2 + ocnt[0] % 4
            ocnt[0] += 1
            tr_multi([(ps[bank][:, j * 128:(j + 1) * 128], ych(4 * half + j)[:, tk], ident_f[:])
                      for j in range(4)],
                     reads=[ykey(4 * half + j) for j in range(4)] + [("ident_f",)], writes=[("ps", bank)])
            b = state["ost"] % 2
            state["ost"] += 1
            if ocnt[0] % 2 == 0:
                P.add("act", lambda e, b=b, bank=bank: e.copy(out=ost[:, b, :], in_=ps[bank][:]),
                      reads=[("ps", bank)], writes=[("ost", b)])
            else:
                P.add("dve", lambda e, b=b, bank=bank: e.tensor_copy(out=ost[:, b, :], in_=ps[bank][:]),
                      reads=[("ps", bank)], writes=[("ost", b)])
            t0 = p * T + s * 128
            dst = y_d[t0:t0 + 128, half * 512:(half + 1) * 512]
            P.add("pool", lambda e, b=b, dst=dst: e.dma_start(out=dst, in_=ost[:, b, :]),
                  reads=[("ost", b)], writes=[("ydram", b)], dma_sem=f"ost{b}")

    setup()
    pend = {"b": [issue_xload(0, 0), issue_xload(0, 1)]}

    def prologue_a(p):
        bufs = list(pend["b"])
        for s in range(NSUB):
            if s >= 2:
                bufs.append(issue_xload(p, s))
            x_transpose(s, bufs[s])
        if p + 1 < n_pass:
            pend["b"] = [issue_xload(p + 1, 0), issue_xload(p + 1, 1)]
        for c in range(8):
            norm_sq(c)
            if c > 0:
                norm_mm(c - 1)
        norm_mm(7)

    prologue_a(0)
    norm_finish(CN_F1)
    for p in range(n_pass):
        ffn(s_f1i, s_f1o, "f1i", "f1o")
        norm_finish(CN_MX)
        mixer(p)
        norm_finish(CN_F2)
        ffn(s_f2i, s_f2o, "f2i", "f2o")
        norm_finish(CN_FIN, final=True)
        if p + 1 < n_pass:
            prologue_a(p + 1)
        output(p, 1)
        if p + 1 < n_pass:
            norm_finish(CN_F1)
        output(p, 0)

    P.emit(nc, es, final_wait_sems=("ost0", "ost1"))
    es.close()
    return nc


def _colmat(v):
    v = np.asarray(v, np.float32).reshape(-1, 128)
    return v.T


def make_in_maps(inputs, n_tok=SEQ, n_cores=8):
    f = lambda k: np.ascontiguousarray(np.asarray(inputs[k], np.float32))
    cols = np.zeros((128, 64), np.float32)
    cols[:, 0:8] = _colmat(f("ffn1_norm")[0])
    cols[:, 8:16] = _colmat(f("mix_norm")[0])
    cols[:, 16:24] = _colmat(f("ffn2_norm")[0])
    cols[:, 24:32] = _colmat(f("final_norm"))
    cols[:, 32:48] = _colmat(f("b_gate")[0])
    cols[:, 48:56] = _colmat(f("b_head_norm")[0].reshape(-1))
    pidx = np.arange(128)
    cols[:, 56] = (pidx < 17)
    cols[:, 57] = (pidx == 17)
    cols[:, 58] = (pidx == 0)
    cols[:, 59] = (pidx == 1)
    cols[:, 60] = EPS
    cols[:, 61] = 1.0
    rows = np.empty((128, 2048), np.float32)
    rows[:, 0:1024] = f("a_ln_gain")[0][None, :]
    rows[:, 1024:2048] = f("a_ln_bias")[0][None, :]
    wst = np.ascontiguousarray(f("a_w_s")[0].transpose(2, 0, 1)).reshape(128, 1024)
    bs2 = np.ascontiguousarray(np.broadcast_to(f("a_b_s")[0].reshape(1, 1024), (2, 1024)))
    wal = np.empty((18, 512), np.float32)
    wal[0:16] = f("b_w_alpha")[0]
    wal[16] = f("b_b_alpha")[0]
    wal[17] = f("b_b_alpha")[0]
    ident = np.eye(128, dtype=np.float32)
    umask = np.triu(np.ones((128, 128), np.float32))
    shared = {
        "ffn1_w_in": f("ffn1_w_in")[0], "ffn1_w_out": f("ffn1_w_out")[0], "w_in": f("w_in")[0],
        "w_proj_a": f("w_proj_a")[0], "w_proj_b": f("w_proj_b")[0], "w_o": f("w_o")[0],
        "ffn2_w_in": f("ffn2_w_in")[0], "ffn2_w_out": f("ffn2_w_out")[0],
        "cols": cols, "rows_bc": rows, "wst": wst, "bs2": bs2, "wal18": wal, "ident": ident, "umask": umask,
    }
    x = f("x")
    maps = []
    for c in range(n_cores):
        m = dict(shared)
        m["x"] = np.ascontiguousarray(x[c, :n_tok, :])
        maps.append(m)
    return maps


_NC_CACHE = {}


def kernel(**inputs):
    n_tok = SEQ
    if n_tok not in _NC_CACHE:
        _NC_CACHE[n_tok] = build_nc(n_tok)
    nc = _NC_CACHE[n_tok]
    in_maps = make_in_maps(inputs, n_tok, 8)
    res = run_bass_kernel_spmd(nc, in_maps, core_ids=list(range(8)))
    out = np.stack([np.asarray(r["y"], np.float32) for r in res.results], axis=0)
    return out
```
